# Optimizing a Trainium2 kernel written in Bass

```python
import jax
import jax.numpy as jnp
from jax import lax
import numpy as np

D_MODEL = 4096
BATCH = 2
SEQ = 8192
DEPTH = 4

N_EVEN = (DEPTH + 1) // 2
N_ODD = DEPTH // 2
ATTN_WIDTH = D_MODEL // 2
HEAD_DIM = 64
N_Q_HEADS = ATTN_WIDTH // HEAD_DIM
N_KV_HEADS = N_Q_HEADS // 8
KV_WIDTH = N_KV_HEADS * HEAD_DIM
WINDOW = 128
ROT_DIM = HEAD_DIM // 4
ROPE_THETA = 500000.0
MASK_VALUE = -1e9
HGRN_WIDTH = D_MODEL - ATTN_WIDTH
HGRN_DIM = 128
N_HGRN_HEADS = HGRN_WIDTH // HGRN_DIM
HGRN_CHUNK = 64
EVEN_IN_COLS = ATTN_WIDTH + 2 * KV_WIDTH + 4 * HGRN_WIDTH
LRU_WIDTH = D_MODEL
N_LRU_HEADS = 16
LRU_HEAD_DIM = LRU_WIDTH // N_LRU_HEADS
CONV_WIDTH = 4
RG_C = 8.0
N_GROUPS = 4
EXPERTS_PER_GROUP = 8
N_EXPERTS = N_GROUPS * EXPERTS_PER_GROUP
TOP_K = 2
EXPERT_FF = 3 * D_MODEL // 32
DISPATCH_BLOCK = 128
ALPHA = (2.0 * DEPTH) ** 0.25
BETA = (8.0 * DEPTH) ** -0.25
LN_EPS = 1e-5
RMS_EPS = 1e-6

kernel_name = 'hybrid_swa_hgrn2_rglru_hmoe_deepnorm'


def _split_cols(t, widths):
    out, off = [], 0
    for w in widths:
        out.append(t[..., off:off + w])
        off += w
    return out


def layer_norm(x, g, b):
    xf = x.astype(jnp.float32)
    mu = jnp.mean(xf, axis=-1, keepdims=True)
    var = jnp.mean(jnp.square(xf - mu), axis=-1, keepdims=True)
    return ((xf - mu) * lax.rsqrt(var + LN_EPS) * g + b).astype(x.dtype)


def partial_rotary(t, cos, sin):
    half = ROT_DIM // 2
    t1, t2, rest = t[..., :half], t[..., half:ROT_DIM], t[..., ROT_DIM:]
    return jnp.concatenate([t1 * cos - t2 * sin, t2 * cos + t1 * sin, rest], axis=-1)


def swa_with_sinks(q, k, v, sinks):
    B, S = q.shape[0], q.shape[1]
    nb = S // WINDOW
    G = N_Q_HEADS // N_KV_HEADS
    qb = q.reshape(B, nb, WINDOW, N_KV_HEADS, G, HEAD_DIM)

    def band(t):
        tb = t.reshape(B, nb, WINDOW, N_KV_HEADS, HEAD_DIM)
        prev = jnp.pad(tb, ((0, 0), (1, 0), (0, 0), (0, 0), (0, 0)))[:, :-1]
        return jnp.concatenate([prev, tb], axis=2)

    kw, vw = band(k), band(v)
    s = jnp.einsum('bnqhgd,bnkhd->bnhgqk', qb, kw) * (HEAD_DIM ** -0.5)
    qpos = jnp.arange(WINDOW)[:, None] + WINDOW
    kpos = jnp.arange(2 * WINDOW)[None, :]
    rel = qpos - kpos
    in_band = (rel >= 0) & (rel < WINDOW)
    has_prev = (jnp.arange(nb) > 0)[:, None, None] | (kpos >= WINDOW)[None]
    mask = (in_band[None] & has_prev)[None, :, None, None]
    s = jnp.where(mask, s, MASK_VALUE)
    sink = sinks.astype(jnp.float32).reshape(1, 1, N_KV_HEADS, G, 1, 1)
    m = jnp.maximum(jnp.max(s, axis=-1, keepdims=True), sink)
    p = jnp.where(mask, jnp.exp(s - m), 0.0)
    denom = jnp.sum(p, axis=-1, keepdims=True) + jnp.exp(sink - m)
    o = jnp.einsum('bnhgqk,bnkhd->bnqhgd', p / denom, vw)
    return o.reshape(B, S, N_Q_HEADS, HEAD_DIM)


def hgrn2_chunked(q, k, v, log_f):
    B, S, H, DK = q.shape
    DV = v.shape[-1]
    nc = S // HGRN_CHUNK

    def to_chunks(t):
        return t.reshape(B, nc, HGRN_CHUNK, H, t.shape[-1]).transpose(1, 0, 3, 2, 4)

    causal = jnp.tril(jnp.ones((HGRN_CHUNK, HGRN_CHUNK), dtype=bool))[:, :, None]

    def step(state, inp):
        qc, kc, vc, lf = inp
        b = jnp.cumsum(lf, axis=2)
        diff = b[:, :, :, None, :] - b[:, :, None, :, :]
        decay = jnp.where(causal, jnp.exp(jnp.where(causal, diff, 0.0)), 0.0)
        scores = jnp.einsum('bhtk,bhsk,bhtsk->bhts', qc, kc, decay)
        o = jnp.einsum('bhts,bhsv->bhtv', scores, vc) + jnp.einsum('bhtk,bhkv->bhtv', qc * jnp.exp(b), state)
        b_last = b[:, :, -1:, :]
        new_state = jnp.exp(b_last[:, :, 0, :])[..., None] * state + jnp.einsum('bhsk,bhsv->bhkv', kc * jnp.exp(b_last - b), vc)
        return new_state, o

    s0 = jnp.zeros((B, H, DK, DV), jnp.float32)
    _, o = lax.scan(step, s0, (to_chunks(q), to_chunks(k), to_chunks(v), to_chunks(log_f)))
    return o.transpose(1, 0, 3, 2, 4).reshape(B, S, H, DV)


def even_mixer(x, cos, sin, w_in, w_out, sinks, lb, gnorm_w):
    B, S, _ = x.shape
    proj = jnp.einsum('bsd,de->bse', x, w_in).astype(jnp.float32)
    q_a, k_a, v_a, q_b, f_b, i_b, g_b = _split_cols(
        proj, (ATTN_WIDTH, KV_WIDTH, KV_WIDTH, HGRN_WIDTH, HGRN_WIDTH, HGRN_WIDTH, HGRN_WIDTH))
    q_a = partial_rotary(q_a.reshape(B, S, N_Q_HEADS, HEAD_DIM), cos, sin)
    k_a = partial_rotary(k_a.reshape(B, S, N_KV_HEADS, HEAD_DIM), cos, sin)
    o_a = swa_with_sinks(q_a, k_a, v_a.reshape(B, S, N_KV_HEADS, HEAD_DIM), sinks)
    def hd(t):
        return t.reshape(B, S, N_HGRN_HEADS, HGRN_DIM)
    lbf = lb.astype(jnp.float32).reshape(N_HGRN_HEADS, HGRN_DIM)
    z = hd(f_b)
    f = lbf + (1.0 - lbf) * jax.nn.sigmoid(z)
    log_f = jnp.log(f)
    k_b = (1.0 - lbf) * jax.nn.sigmoid(-z)
    o_b = hgrn2_chunked(jax.nn.silu(hd(q_b)), k_b, hd(i_b), log_f)
    o_b = o_b * lax.rsqrt(jnp.mean(jnp.square(o_b), axis=-1, keepdims=True) + RMS_EPS) * gnorm_w.astype(jnp.float32) * jax.nn.silu(hd(g_b))
    mixed = jnp.concatenate([o_a.reshape(B, S, ATTN_WIDTH), o_b.reshape(B, S, HGRN_WIDTH)], axis=-1)
    return jnp.einsum('bse,ed->bsd', mixed.astype(x.dtype), w_out)


def rg_lru(u, wa, ba, wx, bx, lam):
    B, S, _ = u.shape
    uh = u.reshape(B, S, N_LRU_HEADS, LRU_HEAD_DIM)
    r = jax.nn.sigmoid(jnp.einsum('bshi,hij->bshj', uh, wa).reshape(B, S, LRU_WIDTH) + ba)
    ig = jax.nn.sigmoid(jnp.einsum('bshi,hij->bshj', uh, wx).reshape(B, S, LRU_WIDTH) + bx)
    log_a = -RG_C * r * jax.nn.softplus(-lam.astype(jnp.float32))
    a = jnp.exp(log_a)
    b = jnp.sqrt(jnp.maximum(-jnp.expm1(2.0 * log_a), 0.0)) * (ig * u)

    def combine(left, right):
        a_l, b_l = left
        a_r, b_r = right
        return a_l * a_r, a_r * b_l + b_r

    _, h = lax.associative_scan(combine, (a, b), axis=1)
    return h


def recurrent_mixer(x, w_in, conv_w, conv_b, wa, ba, wx, bx, lam, w_out):
    B, S, _ = x.shape
    proj = jnp.einsum('bsd,de->bse', x, w_in).astype(jnp.float32)
    gate_br, rnn_br = proj[..., :LRU_WIDTH], proj[..., LRU_WIDTH:]
    gate_br = jax.nn.gelu(gate_br)
    xpad = jnp.pad(rnn_br, ((0, 0), (CONV_WIDTH - 1, 0), (0, 0)))
    conv = conv_b.astype(jnp.float32) + xpad[:, 0:S] * conv_w[0]
    for j in range(1, CONV_WIDTH):
        conv = conv + xpad[:, j:j + S] * conv_w[j]
    h = rg_lru(conv, wa, ba, wx, bx, lam)
    return jnp.einsum('bse,ed->bsd', (h * gate_br).astype(x.dtype), w_out)


def hierarchical_moe(x, wg, bg, we, be, w_gate, w_up, w_down):
    B, S, D = x.shape
    xf = x.reshape(-1, D)
    N = xf.shape[0]
    xr = xf.astype(jnp.float32)
    p_group = jax.nn.softmax(xr @ wg.astype(jnp.float32) + bg, axis=-1)
    p_top, g_sel = lax.top_k(p_group, 1)
    e_logits = (xr @ we.astype(jnp.float32) + be).reshape(N, N_GROUPS, EXPERTS_PER_GROUP)
    e_logits = e_logits[jnp.arange(N), g_sel[:, 0]]
    w_top, e_local = lax.top_k(jax.nn.softmax(e_logits, axis=-1), TOP_K)
    w_top = w_top / jnp.sum(w_top, axis=-1, keepdims=True) * p_top
    e_idx = g_sel * EXPERTS_PER_GROUP + e_local
    A = N * TOP_K
    n_blocks = (A + N_EXPERTS * (DISPATCH_BLOCK - 1) + DISPATCH_BLOCK - 1) // DISPATCH_BLOCK
    n_slots = n_blocks * DISPATCH_BLOCK
    flat_e = e_idx.reshape(-1)
    onehot = jax.nn.one_hot(flat_e, N_EXPERTS, dtype=jnp.int32)
    counts = jnp.sum(onehot, axis=0)
    rank = jnp.sum((jnp.cumsum(onehot, axis=0) - 1) * onehot, axis=1)
    padded = (counts + DISPATCH_BLOCK - 1) // DISPATCH_BLOCK * DISPATCH_BLOCK
    pad_end = jnp.cumsum(padded)
    dest = pad_end[flat_e] - padded[flat_e] + rank
    slot_tok = jnp.full((n_slots,), N, jnp.int32).at[dest].set(jnp.arange(A, dtype=jnp.int32) // TOP_K)
    slot_w = jnp.zeros((n_slots,), jnp.float32).at[dest].set(w_top.reshape(-1))
    block_e = jnp.minimum(jnp.searchsorted(pad_end, jnp.arange(n_blocks) * DISPATCH_BLOCK, side='right'), N_EXPERTS - 1)
    x_pad = jnp.concatenate([xf, jnp.zeros((1, D), xf.dtype)], axis=0)

    def run_block(args):
        toks, e = args
        xb = x_pad[toks]
        hid = jax.nn.silu(xb @ w_gate[e]) * (xb @ w_up[e])
        return hid @ w_down[e]

    y = lax.map(run_block, (slot_tok.reshape(n_blocks, DISPATCH_BLOCK), block_e))
    y = y.reshape(n_slots, D).astype(jnp.float32) * slot_w[:, None]
    out = jax.ops.segment_sum(y, slot_tok, num_segments=N + 1)[:N]
    return out.reshape(B, S, D).astype(x.dtype)


def setup_inputs(seed: int = 0) -> dict:
    key = jax.random.key(seed)
    ks = jax.random.split(key, 32)
    f32 = jnp.float32

    def nrm(k, shape, scale):
        return jax.random.normal(k, shape, f32) * scale

    u = jax.random.uniform(ks[15], (N_ODD, LRU_WIDTH), f32, 0.9, 0.999)
    s_lam = u ** (1.0 / RG_C)
    return {
        'x': nrm(ks[0], (BATCH, SEQ, D_MODEL), 1.0),
        'positions': jnp.broadcast_to(jnp.arange(SEQ, dtype=jnp.int32), (BATCH, SEQ)),
        'even_w_in': nrm(ks[1], (N_EVEN, D_MODEL, EVEN_IN_COLS), D_MODEL ** -0.5),
        'even_w_out': nrm(ks[2], (N_EVEN, ATTN_WIDTH + HGRN_WIDTH, D_MODEL), (ATTN_WIDTH + HGRN_WIDTH) ** -0.5 * BETA),
        'attn_sinks': nrm(ks[3], (N_EVEN, N_Q_HEADS), 1.0),
        'hgrn_lb_logits': nrm(ks[4], (N_EVEN, HGRN_WIDTH), 0.1),
        'hgrn_gnorm_w': 1.0 + nrm(ks[5], (N_EVEN, HGRN_DIM), 0.01),
        'rec_w_in': nrm(ks[6], (N_ODD, D_MODEL, 2 * LRU_WIDTH), D_MODEL ** -0.5),
        'rec_conv_w': nrm(ks[7], (N_ODD, CONV_WIDTH, LRU_WIDTH), CONV_WIDTH ** -0.5),
        'rec_conv_b': nrm(ks[8], (N_ODD, LRU_WIDTH), 0.01),
        'rec_gate_a_w': nrm(ks[9], (N_ODD, N_LRU_HEADS, LRU_HEAD_DIM, LRU_HEAD_DIM), LRU_HEAD_DIM ** -0.5),
        'rec_gate_a_b': nrm(ks[10], (N_ODD, LRU_WIDTH), 0.01),
        'rec_gate_x_w': nrm(ks[11], (N_ODD, N_LRU_HEADS, LRU_HEAD_DIM, LRU_HEAD_DIM), LRU_HEAD_DIM ** -0.5),
        'rec_gate_x_b': nrm(ks[12], (N_ODD, LRU_WIDTH), 0.01),
        'rec_lambda': jnp.log(s_lam) - jnp.log1p(-s_lam),
        'rec_w_out': nrm(ks[13], (N_ODD, LRU_WIDTH, D_MODEL), LRU_WIDTH ** -0.5 * BETA),
        'ln_mix_g': 1.0 + nrm(ks[16], (DEPTH, D_MODEL), 0.01),
        'ln_mix_b': nrm(ks[17], (DEPTH, D_MODEL), 0.01),
        'ln_ffn_g': 1.0 + nrm(ks[18], (DEPTH, D_MODEL), 0.01),
        'ln_ffn_b': nrm(ks[19], (DEPTH, D_MODEL), 0.01),
        'router_group_w': nrm(ks[20], (DEPTH, D_MODEL, N_GROUPS), D_MODEL ** -0.5),
        'router_group_b': nrm(ks[21], (DEPTH, N_GROUPS), 0.01),
        'router_expert_w': nrm(ks[22], (DEPTH, D_MODEL, N_EXPERTS), D_MODEL ** -0.5),
        'router_expert_b': nrm(ks[23], (DEPTH, N_EXPERTS), 0.01),
        'moe_w_gate': nrm(ks[24], (DEPTH, N_EXPERTS, D_MODEL, EXPERT_FF), D_MODEL ** -0.5),
        'moe_w_up': nrm(ks[25], (DEPTH, N_EXPERTS, D_MODEL, EXPERT_FF), D_MODEL ** -0.5),
        'moe_w_down': nrm(ks[26], (DEPTH, N_EXPERTS, EXPERT_FF, D_MODEL), EXPERT_FF ** -0.5 * BETA),
    }


def reference(x, positions, even_w_in, even_w_out, attn_sinks, hgrn_lb_logits, hgrn_gnorm_w,
              rec_w_in, rec_conv_w, rec_conv_b, rec_gate_a_w, rec_gate_a_b, rec_gate_x_w, rec_gate_x_b,
              rec_lambda, rec_w_out, ln_mix_g, ln_mix_b, ln_ffn_g, ln_ffn_b,
              router_group_w, router_group_b, router_expert_w, router_expert_b,
              moe_w_gate, moe_w_up, moe_w_down):
    inv_freq = ROPE_THETA ** (-jnp.arange(0, ROT_DIM, 2, dtype=jnp.float32) / ROT_DIM)
    ang = positions.astype(jnp.float32)[..., None] * inv_freq
    cos = jnp.cos(ang)[:, :, None, :]
    sin = jnp.sin(ang)[:, :, None, :]
    sm = jax.nn.softmax(hgrn_lb_logits.astype(jnp.float32), axis=0)
    lb_table = jnp.cumsum(sm, axis=0) - sm[:1]
    h = x
    for layer in range(DEPTH):
        j = layer // 2
        if layer % 2 == 0:
            mix = even_mixer(h, cos, sin, even_w_in[j], even_w_out[j], attn_sinks[j], lb_table[j], hgrn_gnorm_w[j])
        else:
            mix = recurrent_mixer(h, rec_w_in[j], rec_conv_w[j], rec_conv_b[j], rec_gate_a_w[j], rec_gate_a_b[j],
                                  rec_gate_x_w[j], rec_gate_x_b[j], rec_lambda[j], rec_w_out[j])
        h = layer_norm(ALPHA * h + mix, ln_mix_g[layer], ln_mix_b[layer])
        ffn = hierarchical_moe(h, router_group_w[layer], router_group_b[layer], router_expert_w[layer],
                               router_expert_b[layer], moe_w_gate[layer], moe_w_up[layer], moe_w_down[layer])
        h = layer_norm(ALPHA * h + ffn, ln_ffn_g[layer], ln_ffn_b[layer])
    return h
```

```python
from contextlib import ExitStack
import numpy as np
import concourse.bass as bass
import concourse.mybir as mybir

F32 = mybir.dt.float32
BF16 = mybir.dt.bfloat16
I32 = mybir.dt.int32
AF = mybir.ActivationFunctionType
ALU = mybir.AluOpType
AX = mybir.AxisListType

SEM_ROLL = 30000


class Prog:
    def __init__(self, nc, ctx, n_dma_sems=48):
        self.nc = nc
        self.ctx = ctx
        self.E = {'pe': nc.tensor, 'act': nc.scalar, 'dve': nc.vector, 'pool': nc.gpsimd, 'sp': nc.sync}
        self.cur_sem = {}
        self.cnt = {}
        self.nsem = 0
        for e in ('pe', 'act', 'dve', 'pool'):
            self._new_eng_sem(e)
        self.dq = {}
        for q, n in (('sp', 20), ('pool', 20), ('act', 6)):
            self.dq[q] = {'sems': [self._alloc_sem('d%s%d' % (q, i)) for i in range(n)], 'val': [0] * n, 'next': 0}
        self.known = {e: {} for e in self.E}
        self.lastw = {}
        self.readers = {}
        self.sem_by_id = {}
        self.n_ins = 0
        self.n_wait = 0

    def _alloc_sem(self, name):
        s = self.ctx.enter_context(self.nc.semaphore(name))
        self.nsem += 1
        return s

    def _new_eng_sem(self, e):
        self.cur_sem[e] = self._alloc_sem('s_%s_%d' % (e, self.nsem))
        self.cnt[e] = 0

    def _wait(self, e, tok):
        sem, val = tok
        k = self.known[e]
        sid = id(sem)
        if k.get(sid, 0) >= val:
            return
        self.E[e].wait_ge(sem, val)
        self.n_wait += 1
        k[sid] = val

    def _deps(self, e, reads, writes, pe_fifo=False):
        toks = []
        for key in reads:
            t = self.lastw.get(key)
            if t is not None:
                toks.append(t)
        for key in writes:
            t = self.lastw.get(key)
            if t is not None:
                toks.append(t)
            toks.extend(self.readers.get(key, ()))
        for t in toks:
            if pe_fifo and t[2] == 'pe':
                continue
            self._wait(e, t[:2])

    def _record(self, tok, reads, writes):
        for key in reads:
            self.readers.setdefault(key, []).append(tok)
        for key in writes:
            self.lastw[key] = tok
            self.readers[key] = []

    def op(self, e, fn, reads=(), writes=()):
        self._deps(e, reads, writes, pe_fifo=(e == 'pe'))
        ins = fn()
        if self.cnt[e] >= SEM_ROLL:
            self._new_eng_sem(e)
        self.cnt[e] += 1
        ins.then_inc(self.cur_sem[e], 1)
        tok = (self.cur_sem[e], self.cnt[e], e)
        self._record(tok, reads, writes)
        self.n_ins += 1
        return tok

    def _dma_common(self, q, issue, reads, writes):
        self._deps(q, reads, writes)
        d = self.dq[q]
        i = d['next']
        d['next'] = (i + 1) % len(d['sems'])
        if d['val'][i] > 0:
            self._wait(q, (d['sems'][i], d['val'][i]))
        if d['val'][i] >= SEM_ROLL:
            d['sems'][i] = self._alloc_sem('dr%d' % self.nsem)
            d['val'][i] = 0
        ins = issue()
        d['val'][i] += 16
        ins.then_inc(d['sems'][i], 16)
        tok = (d['sems'][i], d['val'][i], 'dma')
        self._record(tok, reads, writes)
        self.n_ins += 1
        return tok

    def dma(self, q, out, in_, reads=(), writes=(), **kw):
        return self._dma_common(q, lambda: self.E[q].dma_start(out=out, in_=in_, **kw), reads, writes)

    def raw_dma(self, q, fn, reads=(), writes=()):
        return self._dma_common(q, fn, reads, writes)

    def finish(self, keys):
        for key in keys:
            t = self.lastw.get(key)
            if t is not None:
                self._wait('sp', t[:2])
        for q, d in self.dq.items():
            for sem, val in zip(d['sems'], d['val']):
                if val > 0:
                    self._wait('sp', (sem, val))
        for sem, val in getattr(self, 'retired', []):
            self._wait('sp', (sem, val))

    def sb(self, name, shape, dt):
        return self.ctx.enter_context(self.nc.sbuf_tensor("sb_" + name, shape, dt))

    def ps(self, name, shape, dt=F32):
        return self.ctx.enter_context(self.nc.psum_tensor("pp_" + name, shape, dt))


RG_C = 8.0
GELU_C = 0.7978845608028654


def build_odd(D, T, NH, TT=512):
    KC = D // 128
    CC = NH * 2
    NOC = 2 * CC
    NT = T // TT
    nc = bass.Bass("TRN2", target_bir_lowering=False)
    hT = nc.dram_tensor("hT", [D, T], F32, kind="ExternalInput").ap()
    w = nc.dram_tensor("w", [NOC, 128, KC, 128], F32, kind="ExternalInput").ap()
    vec = nc.dram_tensor("vec", [128, CC, 8], F32, kind="ExternalInput").ap()
    wg = nc.dram_tensor("wg", [128, 2, NH, 2, 256], F32, kind="ExternalInput").ap()
    oT = nc.dram_tensor("oT", [CC * 128, T], BF16, kind="ExternalOutput").ap()
    hT_v = hT.rearrange("(c p) t -> p c t", p=128)
    oT_v = oT.rearrange("(c p) t -> p c t", p=128)
    ctx = ExitStack()
    P = Prog(nc, ctx)
    V, A, G = nc.vector, nc.scalar, nc.gpsimd
    NWB = 3
    hs = [P.sb("hs%d" % i, [128, KC, TT], BF16) for i in range(2)]
    wb = [P.sb("wb%d" % i, [128, KC, 128], BF16) for i in range(NWB)]
    vs = P.sb("vs", [128, CC, 8], F32)
    sc = P.sb("sc", [128, CC, 2], F32)
    wgs = P.sb("wgs", [128, 2, NH, 2, 256], BF16)
    gate = P.sb("gate", [128, CC, TT], F32)
    xb = P.sb("xb", [128, CC, TT + 3], F32)
    cv = P.sb("cv", [128, CC, TT], F32)
    cvb = P.sb("cvb", [128, CC, TT], BF16)
    hst = P.sb("hst", [128, CC, 2], F32)
    tmp = [P.sb("tmp%d" % i, [128, 6, TT], F32) for i in range(2)]
    ob = [P.sb("ob%d" % i, [128, TT], BF16) for i in range(2)]
    pss = [P.ps("ps%d" % i, [128, 512]) for i in range(8)]

    P.dma('sp', vs[:], vec, writes=['vs'])
    P.dma('pool', wgs[:], wg, writes=['wgs'])
    P.op('act', lambda: A.activation(sc[:, :, 1], vs[:, :, 7], AF.Exp, scale=-1.0), reads=['vs'], writes=['sc1'])
    P.op('act', lambda: A.activation(sc[:, :, 0], sc[:, :, 1], AF.Ln, bias=1.0), reads=['sc1'], writes=['sc0'])
    P.op('dve', lambda: V.tensor_scalar_mul(sc[:, :, 0], sc[:, :, 0], -RG_C), reads=['sc0'], writes=['sc0'])
    P.op('dve', lambda: V.memset(xb[:, :, 0:3], 0.0), writes=[('xb', c) for c in range(CC)])
    P.op('dve', lambda: V.memset(hst[:], 0.0), writes=['hst'])

    wcnt = 0
    pcnt = 0
    for ti in range(NT):
        t0 = ti * TT
        hsb = hs[ti % 2]
        P.dma('pool', hsb[:], hT_v[:, :, t0:t0 + TT], writes=[('hs', ti % 2)])
        for oc in range(NOC):
            wbi = wcnt % NWB
            wcnt += 1
            P.dma('pool', wb[wbi][:], w[oc], writes=[('wb', wbi)])
            ps = pss[pcnt % 2]
            pk = ('ps', pcnt % 2)
            pcnt += 1
            for c in range(KC):
                P.op('pe', lambda: nc.tensor.matmul(ps[:], wb[wbi][:, c, :], hsb[:, c, :], start=(c == 0), stop=(c == KC - 1)),
                     reads=[('wb', wbi), ('hs', ti % 2)], writes=[pk])
            if oc < CC:
                cc = oc
                tm = tmp[cc % 2]
                tk = ('tmp', cc % 2)
                P.op('act', lambda: A.activation(tm[:, 0, :], ps[:], AF.Square), reads=[pk], writes=[tk])
                P.op('dve', lambda: V.tensor_scalar(tm[:, 0, :], tm[:, 0, :], 2 * GELU_C * 0.044715, 2 * GELU_C, ALU.mult, ALU.add),
                     reads=[tk], writes=[tk])
                P.op('dve', lambda: V.tensor_tensor(tm[:, 0, :], tm[:, 0, :], ps[:], ALU.mult), reads=[tk, pk], writes=[tk])
                P.op('act', lambda: A.activation(tm[:, 0, :], tm[:, 0, :], AF.Sigmoid), reads=[tk], writes=[tk])
                P.op('dve', lambda: V.tensor_tensor(gate[:, cc, :], tm[:, 0, :], ps[:], ALU.mult), reads=[tk, pk],
                     writes=[('gate', cc)])
            else:
                cc = oc - CC
                P.op('act', lambda: A.copy(xb[:, cc, 3:3 + TT], ps[:]), reads=[pk], writes=[('xb', cc)])
                P.op('dve', lambda: V.tensor_scalar(cv[:, cc, :], xb[:, cc, 0:TT], vs[:, cc, 0:1], vs[:, cc, 4:5], ALU.mult, ALU.add),
                     reads=[('xb', cc), 'vs'], writes=[('cv', cc)])
                for j in range(1, 4):
                    P.op('dve', lambda: V.scalar_tensor_tensor(cv[:, cc, :], xb[:, cc, j:j + TT], vs[:, cc, j:j + 1], cv[:, cc, :],
                                                               ALU.mult, ALU.add),
                         reads=[('xb', cc), 'vs', ('cv', cc)], writes=[('cv', cc)])
                P.op('act', lambda: A.copy(cvb[:, cc, :], cv[:, cc, :]), reads=[('cv', cc)], writes=[('cvb', cc)])
                P.op('pool', lambda: G.tensor_copy(xb[:, cc, 0:3], xb[:, cc, TT:TT + 3]), reads=[('xb', cc)], writes=[('xb', cc)])
        for cc in range(CC):
            hh, jc = cc // 2, cc % 2
            tm = tmp[cc % 2]
            tk = ('tmp', cc % 2)
            psa = pss[2 + (cc % 2) * 2]
            psx = pss[3 + (cc % 2) * 2]
            ka, kx = ('ps', 2 + (cc % 2) * 2), ('ps', 3 + (cc % 2) * 2)
            for gi, (pg, kg) in enumerate(((psa, ka), (psx, kx))):
                for ic in range(2):
                    P.op('pe', lambda: nc.tensor.matmul(pg[:], wgs[:, gi, hh, ic, jc * 128:(jc + 1) * 128], cvb[:, 2 * hh + ic, :],
                                                        start=(ic == 0), stop=(ic == 1)),
                         reads=['wgs', ('cvb', 2 * hh + ic)], writes=[kg])
            r, ig, a, a2, bb, hcur = (tm[:, i, :] for i in range(6))
            P.op('act', lambda: A.activation(r, psa[:], AF.Sigmoid, bias=vs[:, cc, 5:6]), reads=[ka, 'vs'], writes=[tk])
            P.op('act', lambda: A.activation(ig, psx[:], AF.Sigmoid, bias=vs[:, cc, 6:7]), reads=[kx, 'vs'], writes=[tk])
            P.op('act', lambda: A.activation(a, r, AF.Exp, scale=sc[:, cc, 0:1]), reads=[tk, 'sc0'], writes=[tk])
            P.op('dve', lambda: V.tensor_tensor(a2, a, a, ALU.mult), reads=[tk], writes=[tk])
            P.op('dve', lambda: V.tensor_scalar(a2, a2, -1.0, 1.0, ALU.mult, ALU.add), reads=[tk], writes=[tk])
            P.op('dve', lambda: V.tensor_scalar_max(a2, a2, 0.0), reads=[tk], writes=[tk])
            P.op('act', lambda: A.activation(a2, a2, AF.Sqrt), reads=[tk], writes=[tk])
            P.op('pool', lambda: G.tensor_tensor(bb, ig, cv[:, cc, :], ALU.mult), reads=[tk, ('cv', cc)], writes=[tk])
            P.op('dve', lambda: V.tensor_tensor(bb, bb, a2, ALU.mult), reads=[tk], writes=[tk])
            P.op('dve', lambda: V.tensor_tensor_scan(hcur, a, bb, hst[:, cc, (ti % 2):(ti % 2) + 1], ALU.mult, ALU.add),
                 reads=[tk, ('hst', cc, ti % 2)], writes=[tk])
            P.op('act', lambda: A.copy(hst[:, cc, ((ti + 1) % 2):((ti + 1) % 2) + 1], hcur[:, TT - 1:TT]), reads=[tk],
                 writes=[('hst', cc, (ti + 1) % 2)])
            obb = ob[cc % 2]
            P.op('dve', lambda: V.tensor_tensor(obb[:], hcur, gate[:, cc, :], ALU.mult), reads=[tk, ('gate', cc)],
                 writes=[('ob', cc % 2)])
            P.dma('sp', oT_v[:, cc, t0:t0 + TT], obb[:], reads=[('ob', cc % 2)], writes=[('oT', cc, ti)])
    P.finish([('oT', cc, ti) for cc in range(CC) for ti in range(NT)])
    print("odd: ins", P.n_ins, "waits", P.n_wait, "sems", P.nsem)
    ctx.close()
    return nc


def odd_inputs(h_b, g, NH, rec_w_in, conv_w, conv_b, wa, ba, wx, bx, lam):
    D = h_b.shape[1]
    LRU = conv_w.shape[1]
    C = NH * 256
    CC = C // 128
    KC = D // 128
    cols = np.concatenate([np.arange(g * C, (g + 1) * C), LRU + np.arange(g * C, (g + 1) * C)])
    wsl = rec_w_in[:, cols]
    wl = np.ascontiguousarray(wsl.reshape(KC, 128, 2 * CC, 128).transpose(2, 1, 0, 3))
    sl = slice(g * C, (g + 1) * C)
    vecs = np.stack([conv_w[0, sl], conv_w[1, sl], conv_w[2, sl], conv_w[3, sl], conv_b[sl], ba[sl], bx[sl], lam[sl]], axis=-1)
    vecs = np.ascontiguousarray(vecs.reshape(CC, 128, 8).transpose(1, 0, 2))
    wgl = np.stack([wa[g * NH:(g + 1) * NH], wx[g * NH:(g + 1) * NH]], axis=0)
    wgl = np.ascontiguousarray(wgl.reshape(2, NH, 2, 128, 256).transpose(3, 0, 1, 2, 4))
    return {"hT": np.ascontiguousarray(h_b.T), "w": wl, "vec": vecs, "wg": wgl}


LN_EPS = 1e-5


def ln_feature_major(P, nc, buf, KC, TT, ones, gb, gi, bi, pss, pskeys, sq, stat, out_fn):
    V, A, G = nc.vector, nc.scalar, nc.gpsimd
    ps1, ps2 = pss
    k1, k2 = pskeys
    for c in range(KC):
        P.op('pe', lambda: nc.tensor.matmul(ps1[:], ones[:], buf[:, c, :], start=(c == 0), stop=(c == KC - 1)),
             reads=[('buf', c), 'ones'], writes=[k1])
    for c in range(KC):
        s = sq[c % 2]
        P.op('act', lambda: A.activation(s[:], buf[:, c, :], AF.Square), reads=[('buf', c)], writes=[('sq', c % 2)])
        P.op('pe', lambda: nc.tensor.matmul(ps2[:], ones[:], s[:], start=(c == 0), stop=(c == KC - 1)),
             reads=[('sq', c % 2), 'ones'], writes=[k2])
    mean, rstd, nmr = stat[:, 0, :], stat[:, 1, :], stat[:, 2, :]
    P.op('act', lambda: A.copy(mean, ps1[:]), reads=[k1], writes=['stat'])
    P.op('dve', lambda: V.tensor_tensor(nmr, mean, mean, ALU.mult), reads=['stat'], writes=['stat'])
    P.op('dve', lambda: V.tensor_tensor(rstd, ps2[:], nmr, ALU.subtract), reads=[k2, 'stat'], writes=['stat'])
    P.op('dve', lambda: V.tensor_scalar(rstd, rstd, 0.0, LN_EPS, ALU.max, ALU.add), reads=['stat'], writes=['stat'])
    P.op('act', lambda: A.activation(rstd, rstd, AF.Sqrt), reads=['stat'], writes=['stat'])
    P.op('dve', lambda: V.reciprocal(rstd, rstd), reads=['stat'], writes=['stat'])
    P.op('dve', lambda: V.scalar_tensor_tensor(nmr, mean, -1.0, rstd, ALU.mult, ALU.mult), reads=['stat'], writes=['stat'])
    for c in range(KC):
        s = sq[c % 2]
        P.op('pool', lambda: G.tensor_tensor(s[:], buf[:, c, :], rstd, ALU.mult), reads=[('buf', c), 'stat'], writes=[('sq', c % 2)])
        P.op('dve', lambda: V.tensor_tensor(s[:], s[:], nmr, ALU.add), reads=[('sq', c % 2), 'stat'], writes=[('sq', c % 2)])
        out_fn(c, s, ('sq', c % 2))


def build_b(D, NTOK, NG, EPG, FF, alpha, TT=512):
    KC = D // 128
    E = NG * EPG
    FC = FF // 128
    NT = NTOK // TT
    NB = TT // 128
    NR = NG + E
    nc = bass.Bass("TRN2", target_bir_lowering=False)
    mT = nc.dram_tensor("mT", [D, NTOK], BF16, kind="ExternalInput").ap()
    hT = nc.dram_tensor("hT", [D, NTOK], F32, kind="ExternalInput").ap()
    wo = nc.dram_tensor("wo", [KC, 128, KC, 128], F32, kind="ExternalInput").ap()
    gbd = nc.dram_tensor("gb", [128, KC, 4], F32, kind="ExternalInput").ap()
    wrd = nc.dram_tensor("wr", [128, KC, NR], F32, kind="ExternalInput").ap()
    brd = nc.dram_tensor("br", [128, NR], F32, kind="ExternalInput").ap()
    wgu = nc.dram_tensor("wgu", [E, 2, FC, 128, KC, 128], F32, kind="ExternalInput").ap()
    wd = nc.dram_tensor("wd", [E, FC * 128, D], F32, kind="ExternalInput").ap()
    identd = nc.dram_tensor("ident", [128, 128], F32, kind="ExternalInput").ap()
    seld = nc.dram_tensor("sel", [E, E, 128], F32, kind="ExternalInput").ap()
    oT = nc.dram_tensor("oT", [D, NTOK], F32, kind="ExternalOutput").ap()
    mT_v = mT.rearrange("(c p) t -> p c t", p=128)
    hT_v = hT.rearrange("(c p) t -> p c t", p=128)
    oT_v = oT.rearrange("(c p) t -> p c t", p=128)
    ctx = ExitStack()
    P = Prog(nc, ctx)
    V, A, G = nc.vector, nc.scalar, nc.gpsimd
    NWB = 3
    bufA = P.sb("bufA", [128, KC, TT], BF16)
    buf = P.sb("buf", [128, KC, TT], F32)
    wb = [P.sb("wb%d" % i, [128, KC, 128], BF16) for i in range(NWB)]
    wdb = [P.sb("wdb%d" % i, [128, FC, D], BF16) for i in range(2)]
    hin = [P.sb("hin%d" % i, [128, TT], F32) for i in range(2)]
    sq = [P.sb("sq%d" % i, [128, TT], F32) for i in range(2)]
    stat = P.sb("stat", [128, 3, TT], F32)
    gb = P.sb("gbs", [128, KC, 4], F32)
    wr = P.sb("wrs", [128, KC, NR], F32)
    br = P.sb("brs", [128, NR], F32)
    ident = P.sb("idents", [128, 128], F32)
    ones = P.sb("ones", [128, 128], F32)
    selb = [P.sb("selb%d" % i, [E, 128], F32) for i in range(2)]
    rt = P.sb("rt", [128, 16, NR], F32)
    cw = P.sb("cw", [128, E], F32)
    cwT = P.sb("cwT", [E, TT], F32)
    cwb = [P.sb("cwb%d" % i, [128, TT], F32) for i in range(2)]
    sg = [P.sb("sg%d" % i, [128, TT], F32) for i in range(2)]
    hid = [P.sb("hid%d" % i, [128, FC, TT], BF16) for i in range(2)]
    pss = [P.ps("ps%d" % i, [128, 512]) for i in range(8)]

    P.dma('sp', gb[:], gbd, writes=['gb'])
    P.dma('sp', wr[:], wrd, writes=['wr'])
    P.dma('sp', br[:], brd, writes=['br'])
    P.dma('sp', ident[:], identd, writes=['ident'])
    P.op('dve', lambda: V.memset(ones[:], 1.0 / D), writes=['ones'])

    wcnt = [0]

    def load_w(src):
        i = wcnt[0] % NWB
        wcnt[0] += 1
        P.dma('pool', wb[i][:], src, writes=[('wb', i)])
        return i

    hcnt = 0
    ocnt = 0
    wdcnt = 0
    for ti in range(NT):
        t0 = ti * TT
        P.dma('pool', bufA[:], mT_v[:, :, t0:t0 + TT], writes=[('bufA', c) for c in range(KC)])
        for oc in range(KC):
            wi = load_w(wo[oc])
            hb = hin[hcnt % 2]
            hk = ('hin', hcnt % 2)
            hcnt += 1
            P.dma('sp', hb[:], hT_v[:, oc, t0:t0 + TT], writes=[hk])
            ps = pss[oc % 2]
            pk = ('ps', oc % 2)
            for c in range(KC):
                P.op('pe', lambda: nc.tensor.matmul(ps[:], wb[wi][:, c, :], bufA[:, c, :], start=(c == 0), stop=(c == KC - 1)),
                     reads=[('wb', wi), ('bufA', c)], writes=[pk])
            P.op('dve', lambda: V.scalar_tensor_tensor(buf[:, oc, :], hb[:], alpha, ps[:], ALU.mult, ALU.add),
                 reads=[hk, pk], writes=[('buf', oc)])

        def out1(c, s, sk):
            P.op('act', lambda: A.activation(bufA[:, c, :], s[:], AF.Identity, bias=gb[:, c, 1:2], scale=gb[:, c, 0:1]),
                 reads=[sk, 'gb'], writes=[('bufA', c)])
            P.op('dve', lambda: V.tensor_scalar(s[:], s[:], gb[:, c, 0:1], gb[:, c, 1:2], ALU.mult, ALU.add),
                 reads=[sk, 'gb'], writes=[sk])
            P.op('pool', lambda: G.tensor_scalar(buf[:, c, :], s[:], alpha, None, ALU.mult), reads=[sk], writes=[('buf', c)])

        ln_feature_major(P, nc, buf, KC, TT, ones, gb, 0, 1, (pss[6], pss[7]), (('ps', 6), ('ps', 7)), sq, stat, out1)

        for tb in range(NB):
            psr = pss[6]
            kr = ('ps', 6)
            for c in range(KC):
                P.op('pe', lambda: nc.tensor.matmul(psr[:, 0:NR], buf[:, c, tb * 128:(tb + 1) * 128], wr[:, c, :],
                                                    start=(c == 0), stop=(c == KC - 1)),
                     reads=[('buf', c), 'wr'], writes=[kr])
            R_ = lambda i, n: rt[:, i, 0:n]
            lg = rt[:, 0, :]
            K = ['rt']
            dv = lambda fn, extra=(): P.op('dve', fn, reads=K + list(extra), writes=K)
            dv(lambda: V.scalar_tensor_tensor(lg, psr[:, 0:NR], 1.0 / alpha, br[:], ALU.mult, ALU.add), [kr, 'br'])
            gl = rt[:, 0, 0:NG]
            el = rt[:, 0, NG:NR]
            gmax, ngmax, gsum, ptop = (rt[:, 1, i:i + 1] for i in range(4))
            m1, m2, dd, w1, w2 = (rt[:, 1, i:i + 1] for i in range(4, 9))
            og = R_(2, NG)
            ge = R_(3, NG)
            e8, o1, e8b, o2, c8 = (R_(i, EPG) for i in range(4, 9))
            dv(lambda: V.reduce_max(gmax, gl, AX.X))
            dv(lambda: V.tensor_scalar(ngmax, gmax, -1.0, None, ALU.mult))
            P.op('act', lambda: A.activation(ge, gl, AF.Exp, bias=ngmax, accum_out=gsum), reads=K, writes=K)
            dv(lambda: V.reciprocal(ptop, gsum))
            dv(lambda: V.tensor_scalar(og, gl, gmax, None, ALU.is_equal))
            dv(lambda: V.tensor_scalar(e8, el[:, 0:EPG], og[:, 0:1], None, ALU.mult))
            for g in range(1, NG):
                dv(lambda: V.scalar_tensor_tensor(e8, el[:, g * EPG:(g + 1) * EPG], og[:, g:g + 1], e8, ALU.mult, ALU.add))
            dv(lambda: V.reduce_max(m1, e8, AX.X))
            dv(lambda: V.tensor_scalar(o1, e8, m1, None, ALU.is_equal))
            dv(lambda: V.scalar_tensor_tensor(e8b, o1, -1e30, e8, ALU.mult, ALU.add))
            dv(lambda: V.reduce_max(m2, e8b, AX.X))
            dv(lambda: V.tensor_scalar(o2, e8b, m2, None, ALU.is_equal))
            dv(lambda: V.tensor_tensor(dd, m2, m1, ALU.subtract))
            P.op('act', lambda: A.activation(dd, dd, AF.Exp), reads=K, writes=K)
            dv(lambda: V.tensor_scalar(w1, dd, 1.0, None, ALU.add))
            dv(lambda: V.reciprocal(w1, w1))
            dv(lambda: V.tensor_tensor(w2, dd, w1, ALU.mult))
            dv(lambda: V.tensor_tensor(w1, w1, ptop, ALU.mult))
            dv(lambda: V.tensor_tensor(w2, w2, ptop, ALU.mult))
            dv(lambda: V.tensor_scalar(c8, o1, w1, None, ALU.mult))
            dv(lambda: V.scalar_tensor_tensor(c8, o2, w2, c8, ALU.mult, ALU.add))
            for g in range(NG):
                P.op('dve', lambda: V.tensor_scalar(cw[:, g * EPG:(g + 1) * EPG], c8, og[:, g:g + 1], None, ALU.mult),
                     reads=K, writes=['cw'])
            pst = pss[7]
            kt = ('ps', 7)
            P.op('pe', lambda: nc.tensor.transpose(pst[0:E, 0:128], cw[:], ident[:]), reads=['cw', 'ident'], writes=[kt])
            P.op('act', lambda: A.copy(cwT[:, tb * 128:(tb + 1) * 128], pst[0:E, 0:128]), reads=[kt], writes=['cwT'])

        for e in range(E):
            psb = pss[6]
            sb_ = selb[e % 2]
            P.dma('sp', sb_[:], seld[:, e, :], writes=[('selb', e % 2)])
            P.op('pe', lambda: nc.tensor.matmul(psb[:], sb_[:], cwT[:], start=True, stop=True),
                 reads=[('selb', e % 2), 'cwT'], writes=[('ps', 6)])
            cb = cwb[e % 2]
            P.op('act', lambda: A.copy(cb[:], psb[:]), reads=[('ps', 6)], writes=[('cwb', e % 2)])
            hd = hid[e % 2]
            wdi = wdcnt % 2
            wdcnt += 1
            P.dma('pool', wdb[wdi][:], wd[e].rearrange("(s p) d -> p s d", p=128), writes=[('wdb', wdi)])
            for fc in range(FC):
                wgi = load_w(wgu[e, 0, fc])
                wui = load_w(wgu[e, 1, fc])
                pg, pu = pss[fc % 2], pss[2 + fc % 2]
                kg, ku = ('ps', fc % 2), ('ps', 2 + fc % 2)
                for c in range(KC):
                    P.op('pe', lambda: nc.tensor.matmul(pg[:], wb[wgi][:, c, :], bufA[:, c, :], start=(c == 0), stop=(c == KC - 1)),
                         reads=[('wb', wgi), ('bufA', c)], writes=[kg])
                for c in range(KC):
                    P.op('pe', lambda: nc.tensor.matmul(pu[:], wb[wui][:, c, :], bufA[:, c, :], start=(c == 0), stop=(c == KC - 1)),
                         reads=[('wb', wui), ('bufA', c)], writes=[ku])
                s_ = sg[fc % 2]
                sk = ('sg', fc % 2)
                P.op('act', lambda: A.activation(s_[:], pg[:], AF.Silu), reads=[kg], writes=[sk])
                P.op('dve', lambda: V.tensor_tensor(s_[:], s_[:], pu[:], ALU.mult), reads=[sk, ku], writes=[sk])
                P.op('pool', lambda: G.tensor_tensor(hd[:, fc, :], s_[:], cb[:], ALU.mult), reads=[sk, ('cwb', e % 2)],
                     writes=[('hid', e % 2, fc)])
            for dc in range(KC):
                py = pss[4 + dc % 2]
                ky = ('ps', 4 + dc % 2)
                for fc in range(FC):
                    P.op('pe', lambda: nc.tensor.matmul(py[:], wdb[wdi][:, fc, dc * 128:(dc + 1) * 128], hd[:, fc, :],
                                                        start=(fc == 0), stop=(fc == FC - 1)),
                         reads=[('wdb', wdi), ('hid', e % 2, fc)], writes=[ky])
                P.op('dve', lambda: V.tensor_tensor(buf[:, dc, :], buf[:, dc, :], py[:], ALU.add), reads=[('buf', dc), ky],
                     writes=[('buf', dc)])

        def out2(c, s, sk):
            nonlocal ocnt
            ob = sg[ocnt % 2]
            ok = ('sg', ocnt % 2)
            ocnt += 1
            P.op('act', lambda: A.activation(ob[:], s[:], AF.Identity, bias=gb[:, c, 3:4], scale=gb[:, c, 2:3]),
                 reads=[sk, 'gb'], writes=[ok])
            P.dma('sp', oT_v[:, c, t0:t0 + TT], ob[:], reads=[ok], writes=[('oT', c, ti)])

        ln_feature_major(P, nc, buf, KC, TT, ones, gb, 2, 3, (pss[6], pss[7]), (('ps', 6), ('ps', 7)), sq, stat, out2)
    P.finish([('oT', c, ti) for c in range(KC) for ti in range(NT)])
    print("B: ins", P.n_ins, "waits", P.n_wait, "sems", P.nsem)
    ctx.close()
    return nc


def b_consts(E):
    ident = np.eye(128, dtype=np.float32)
    sel = np.zeros((E, E, 128), np.float32)
    for e in range(E):
        sel[e, e, :] = 1.0
    return ident, sel


def b_weights(w_out, ln_mix_g, ln_mix_b, ln_ffn_g, ln_ffn_b, wg_r, bg_r, we_r, be_r, w_gate, w_up, w_down):
    D = w_out.shape[1]
    KC = D // 128
    E, _, FF = w_gate.shape
    FC = FF // 128
    wo = np.ascontiguousarray(w_out.reshape(KC, 128, KC, 128).transpose(2, 1, 0, 3))
    gb = np.stack([ln_mix_g, ln_mix_b, ln_ffn_g, ln_ffn_b], axis=-1)
    gb = np.ascontiguousarray(gb.reshape(KC, 128, 4).transpose(1, 0, 2))
    wr = np.concatenate([wg_r, we_r], axis=1)
    NR = wr.shape[1]
    wr = np.ascontiguousarray(wr.reshape(KC, 128, NR).transpose(1, 0, 2))
    br = np.ascontiguousarray(np.broadcast_to(np.concatenate([bg_r, be_r])[None, :], (128, NR)))
    wgu = np.stack([w_gate, w_up], axis=1)
    wgu = np.ascontiguousarray(wgu.reshape(E, 2, KC, 128, FC, 128).transpose(0, 1, 4, 3, 2, 5))
    ident, sel = b_consts(E)
    return {"wo": wo, "gb": gb, "wr": wr, "br": br, "wgu": wgu, "wd": np.ascontiguousarray(w_down), "ident": ident, "sel": sel}

import math

RMS_EPS = 1e-6
ROPE_THETA = 500000.0
NBLK = 22


def build_even(D, T, TT=512, stage=9):
    KC = D // 128
    NT = T // TT
    NB = TT // 128
    NCH = TT // 64
    nc = bass.Bass("TRN2", target_bir_lowering=False)
    dt_in = lambda n, s, d=F32: nc.dram_tensor(n, s, d, kind="ExternalInput").ap()
    hT = dt_in("hT", [D, T])
    w = dt_in("w", [NBLK, 128, KC, 128])
    pos = dt_in("pos", [1, T], I32)
    cstd = dt_in("cst", [128, 16])
    lbld = dt_in("lbl", [128, 4, 2])
    gwd = dt_in("gw", [128, 128])
    permd = dt_in("perm", [128, 128])
    identd = dt_in("identb", [128, 128])
    maskd = dt_in("masks", [128, 2, 256])
    trid = dt_in("tri", [128, 64])
    smaskd = dt_in("smask", [128, TT])
    mo = nc.dram_tensor("mo", [T, 1024], BF16, kind="ExternalOutput").ap()
    hT_v = hT.rearrange("(c p) t -> p c t", p=128)
    ctx = ExitStack()
    P = Prog(nc, ctx)
    V, A, G, PE = nc.vector, nc.scalar, nc.gpsimd, nc.tensor
    NWB = 3
    hs = P.sb("hs", [128, KC, TT], BF16)
    wb = [P.sb("wb%d" % i, [128, KC, 128], BF16) for i in range(NWB)]
    cst = P.sb("cst", [128, 16], F32)
    lbl = P.sb("lbl", [128, 4, 2], F32)
    lbv = P.sb("lbv", [128, 4, 4], F32)
    nsink = P.sb("nsink", [128, 8], F32)
    gwb = P.sb("gwb", [128, 128], F32)
    perm = P.sb("perm", [128, 128], F32)
    identb = P.sb("identb", [128, 128], BF16)
    maskb = P.sb("maskb", [128, 2, 256], BF16)
    tri = P.sb("tri", [128, 64], F32)
    smask = P.sb("smask", [128, TT], F32)
    posi = P.sb("posi", [128, TT], I32)
    cosF = P.sb("cosF", [128, TT], F32)
    sinF = P.sb("sinF", [128, TT], F32)
    tmp = [P.sb("tmp%d" % i, [128, TT], F32) for i in range(10)]
    qf = [P.sb("qf%d" % i, [128, TT], F32) for i in range(2)]
    qrot = P.sb("qrot", [128, 4, TT], BF16)
    krot = P.sb("krot", [128, 128 + TT], BF16)
    vA = P.sb("vA", [128, NB + 1, 64], BF16)
    hv = P.sb("hv", [128, NB, 512], BF16)
    ggw = P.sb("ggw", [128, NB, 512], F32)
    gs = [P.sb("gs%d" % i, [128, 128], F32) for i in range(2)]
    qs = P.sb("qs", [128, 4, TT], F32)
    sig = P.sb("sig", [128, 4, TT], F32)
    qt = P.sb("qt", [128, 4, TT], BF16)
    kt = P.sb("kt", [128, 4, TT], BF16)
    kh = P.sb("kh", [128, 4, TT], BF16)
    qhz = P.sb("qhz", [128, 4, NB, 2, 128], BF16)
    dec = P.sb("dec", [128, 4, NCH], F32)
    khT = [P.sb("khT%d" % i, [128, 128], BF16) for i in range(2)]
    S = P.sb("S", [128, 4, 128], F32)
    Sb = P.sb("Sb", [128, 4, 2, 128], BF16)
    ATm = [P.sb("ATm%d" % i, [128, 128], BF16) for i in range(2)]
    psb_ = [P.sb("p%d" % i, [128, 256], BF16) for i in range(2)]
    pTs = [P.sb("pT%d" % i, [128, 2, 128], BF16) for i in range(2)]
    st = [P.sb("st%d" % i, [128, 8], F32) for i in range(2)]
    junk = P.sb("junk", [128, 128], F32)
    ob = [P.sb("ob%d" % i, [128, 1024], BF16) for i in range(2)]
    pb = [P.ps("pb%d" % i, [128, 1024], BF16) if i in (1, 4) else P.ps("pb%d" % i, [128, 512]) for i in range(8)]
    BK = lambda i: ('pb', i)

    P.dma('sp', cst[:], cstd, writes=['cst'])
    P.dma('sp', lbl[:], lbld, writes=['lbl'])
    P.dma('sp', gwb[:], gwd, writes=['gwb'])
    P.dma('sp', perm[:], permd, writes=['perm'])
    P.dma('sp', tri[:], trid, writes=['tri'])
    P.dma('sp', smask[:], smaskd, writes=['smask'])
    P.dma('pool', identb[:], identd, writes=['identb'])
    P.dma('pool', maskb[:], maskd, writes=['maskb'])
    lb, oml, noml, ltmp = (lbv[:, :, i] for i in range(4))
    P.op('dve', lambda: V.tensor_tensor(ltmp, lbl[:, :, 1], lbl[:, :, 0], ALU.subtract), reads=['lbl'], writes=['lbv'])
    P.op('act', lambda: A.activation(lb, ltmp, AF.Sigmoid), reads=['lbv'], writes=['lbv'])
    P.op('dve', lambda: V.tensor_scalar(lb, lb, cst[:, 10:11], None, ALU.mult), reads=['lbv', 'cst'], writes=['lbv'])
    P.op('dve', lambda: V.tensor_scalar(oml, lb, -1.0, 1.0, ALU.mult, ALU.add), reads=['lbv'], writes=['lbv'])
    P.op('dve', lambda: V.tensor_scalar(noml, oml, -1.0, None, ALU.mult), reads=['lbv'], writes=['lbv'])
    P.op('dve', lambda: V.tensor_scalar(nsink[:], cst[:, 2:10], -1.0, None, ALU.mult), reads=['cst'], writes=['nsink'])
    P.op('dve', lambda: V.memset(krot[:, 0:128], 0.0), writes=['krot'])
    P.op('dve', lambda: V.memset(vA[:, 0, :], 0.0), writes=[('vA', 0)])
    P.op('dve', lambda: V.memset(S[:], 0.0), writes=[('S', h) for h in range(4)])
    P.op('dve', lambda: V.memset(Sb[:], 0.0), writes=[('Sb', h, p) for h in range(4) for p in range(2)])
    for i in range(2):
        P.op('pool', lambda: G.memset(ATm[i][:], 0.0), writes=[('ATm', i)])
    P.op('pool', lambda: G.memset(qhz[:], 0.0), writes=[('qhz', h) for h in range(4)])

    wcnt = [0]
    acnt = [0]
    C1 = 6.28125
    C2 = 2 * math.pi - C1

    def load_w(blk):
        i = wcnt[0] % NWB
        wcnt[0] += 1
        P.dma('pool', wb[i][:], w[blk], writes=[('wb', i)])
        return i

    def proj_fm(blk):
        wi = load_w(blk)
        r = (0, 2)[acnt[0] % 2]
        acnt[0] += 1
        ps, pk = pb[r], BK(r)
        for c in range(KC):
            P.op('pe', lambda: PE.matmul(ps[:], wb[wi][:, c, :], hs[:, c, :], start=(c == 0), stop=(c == KC - 1)),
                 reads=[('wb', wi), 'hs'], writes=[pk])
        return ps, pk

    tmcnt = [0]

    def proj_tm(wi, tb, ncols=128):
        r = (7, 6)[tmcnt[0] % 2]
        tmcnt[0] += 1
        ps, pk = pb[r][:, 0:ncols], BK(r)
        for c in range(KC):
            P.op('pe', lambda: PE.matmul(ps, hs[:, c, tb * 128:(tb + 1) * 128], wb[wi][:, c, 0:ncols], start=(c == 0), stop=(c == KC - 1)),
                 reads=[('wb', wi), 'hs'], writes=[pk])
        return ps, pk

    def sin_table(dst, dkey, shift, t_ang, t_k, kk):
        x, k = tmp[8], tmp[9]
        K = ['rot']
        dv = lambda fn, rd=(), wr=(): P.op('dve', fn, reads=K + list(rd), writes=K + list(wr))
        dv(lambda: V.tensor_scalar(x[:], t_ang[:], shift, None, ALU.add), rd=[kk])
        dv(lambda: V.tensor_scalar(k[:], x[:], 1.0 / (2 * math.pi), None, ALU.mult))
        dv(lambda: V.tensor_copy(posi[:], k[:]), wr=['posi'])
        dv(lambda: V.tensor_copy(k[:], posi[:]), rd=['posi'])
        dv(lambda: V.scalar_tensor_tensor(x[:], k[:], -C1, x[:], ALU.mult, ALU.add))
        dv(lambda: V.scalar_tensor_tensor(x[:], k[:], -C2, x[:], ALU.mult, ALU.add))
        dv(lambda: V.tensor_scalar(k[:], x[:], math.pi, None, ALU.is_gt))
        dv(lambda: V.scalar_tensor_tensor(x[:], k[:], -2 * math.pi, x[:], ALU.mult, ALU.add))
        dv(lambda: V.tensor_scalar(x[:], x[:], math.pi, -math.pi, ALU.min, ALU.max))
        P.op('act', lambda: A.activation(dst[:], x[:], AF.Sin), reads=K, writes=[dkey])

    qfc = [0]
    hb3 = lambda ap: ap.rearrange("p (c t) -> p c t", t=64)

    for ti in range(NT):
        t0 = ti * TT
        if stage < 1:
            break
        P.dma('pool', hs[:], hT_v[:, :, t0:t0 + TT], writes=['hs'])
        P.dma('sp', posi[:], pos[:, t0:t0 + TT].partition_broadcast(128), writes=['posi'])
        ang = tmp[7]
        P.op('dve', lambda: V.tensor_copy(ang[:], posi[:]), reads=['posi'], writes=['ang'])
        P.op('dve', lambda: V.tensor_scalar(ang[:], ang[:], cst[:, 0:1], None, ALU.mult), reads=['ang', 'cst'], writes=['ang'])
        sin_table(sinF, 'sinF', 0.0, ang, None, 'ang')
        P.op('dve', lambda: V.tensor_scalar(sinF[:], sinF[:], cst[:, 1:2], None, ALU.mult), reads=['sinF', 'cst'], writes=['sinF'])
        sin_table(cosF, 'cosF', math.pi / 2, ang, None, 'ang')

        if stage == 1:
            continue
        DBG = 0
        for blk in range(5):
            ps, pk = proj_fm(blk)
            r = qfc[0] % 2
            qfc[0] += 1
            q_, qk = qf[r], ('qf', r)
            P.op('act', lambda: A.copy(q_[:], ps[:]), reads=[pk], writes=[qk])
            t2 = tmp[5]
            if DBG == 1:
                P.op('dve', lambda: V.tensor_tensor(t2[:], q_[:], sinF[:], ALU.mult), reads=[qk, 'sinF'], writes=[('tmp', 5)])
            else:
                P.op('pe', lambda: PE.matmul(pb[3][:], perm[:], q_[:], start=True, stop=True), reads=['perm', qk], writes=[BK(3)])
                P.op('dve', lambda: V.tensor_tensor(t2[:], pb[3][:], sinF[:], ALU.mult), reads=[BK(3), 'sinF'], writes=[('tmp', 5)])
            if DBG != 3:
                P.op('dve', lambda: V.tensor_tensor(q_[:], q_[:], cosF[:], ALU.mult), reads=[qk, 'cosF'], writes=[qk])
            else:
                P.op('pool', lambda: G.tensor_tensor(q_[:], q_[:], cosF[:], ALU.mult), reads=[qk, 'cosF'], writes=[qk])
            if blk < 4:
                P.op('pool', lambda: G.tensor_tensor(qrot[:, blk, :], q_[:], t2[:], ALU.add), reads=[qk, ('tmp', 5)], writes=[('qrot', blk)])
            else:
                P.op('pool', lambda: G.tensor_tensor(krot[:, 128:128 + TT], q_[:], t2[:], ALU.add), reads=[qk, ('tmp', 5)], writes=['krot'])
        if stage < 3:
            continue
        for hh in range(4):
            ps, pk = proj_fm(5 + hh)
            P.op('act', lambda: A.activation(qs[:, hh, :], ps[:], AF.Silu), reads=[pk], writes=[('qs', hh)])
        for hh in range(4):
            ps, pk = proj_fm(9 + hh)
            P.op('act', lambda: A.activation(sig[:, hh, :], ps[:], AF.Sigmoid), reads=[pk], writes=[('sig', hh)])
        for hh in range(4):
            wi = load_w(13 + hh)
            for tb in range(NB):
                ps, pk = proj_tm(wi, tb)
                P.op('act', lambda: A.copy(hv[:, tb, hh * 128:(hh + 1) * 128], ps), reads=[pk], writes=[('hv', tb, hh)])
        gc = 0
        for hh in range(4):
            wi = load_w(17 + hh)
            for tb in range(NB):
                ps, pk = proj_tm(wi, tb)
                g_, gk = gs[gc % 2], ('gs', gc % 2)
                gc += 1
                P.op('act', lambda: A.activation(g_[:], ps, AF.Silu), reads=[pk], writes=[gk])
                P.op('pool', lambda: G.tensor_tensor(ggw[:, tb, hh * 128:(hh + 1) * 128], g_[:], gwb[:], ALU.mult), reads=[gk, 'gwb'],
                     writes=[('ggw', tb, hh)])
        wi = load_w(21)
        for tb in range(NB):
            ps, pk = proj_tm(wi, tb, 64)
            P.op('act', lambda: A.copy(vA[:, tb + 1, :], ps), reads=[pk], writes=[('vA', tb + 1)])

        if stage < 4:
            continue
        for hh in range(4):
            o5 = (hh % 2) * 0
            T1, T2, T3, T4, T5 = (tmp[i] for i in range(5))
            K1, K2, K3, K4, K5 = (('tmp', i) for i in range(5))
            P.op('dve', lambda: V.tensor_scalar(T1[:], sig[:, hh, :], lbv[:, hh, 1:2], lbv[:, hh, 0:1], ALU.mult, ALU.add),
                 reads=[('sig', hh), 'lbv'], writes=[K1])
            P.op('act', lambda: A.activation(T1[:], T1[:], AF.Ln), reads=[K1], writes=[K1])
            P.op('pool', lambda: G.tensor_scalar(T2[:], sig[:, hh, :], lbv[:, hh, 2:3], lbv[:, hh, 1:2], ALU.mult, ALU.add),
                 reads=[('sig', hh), 'lbv'], writes=[K2])
            P.op('dve', lambda: V.tensor_tensor_scan(T3[:], smask[:], T1[:], 0.0, ALU.mult, ALU.add), reads=['smask', K1], writes=[K3])
            b3 = hb3(T3[:])
            P.op('dve', lambda: V.tensor_tensor(hb3(T4[:]), b3, b3[:, :, 31:32].broadcast_to([128, NCH, 64]), ALU.subtract),
                 reads=[K3], writes=[K4])
            P.op('act', lambda: A.activation(T5[:], T4[:], AF.Exp), reads=[K4], writes=[K5])
            P.op('pool', lambda: G.tensor_tensor(qt[:, hh, :], qs[:, hh, :], T5[:], ALU.mult), reads=[('qs', hh), K5], writes=[('qt', hh)])
            P.op('act', lambda: A.activation(T5[:], T4[:], AF.Exp, scale=-1.0), reads=[K4, K5], writes=[K5])
            P.op('dve', lambda: V.tensor_tensor(kt[:, hh, :], T2[:], T5[:], ALU.mult), reads=[K2, K5], writes=[('kt', hh)])
            P.op('act', lambda: A.activation(T5[:], T3[:], AF.Exp), reads=[K3, K5], writes=[K5])
            q4 = qs[:, hh, :].rearrange("p (b two t) -> p b two t", two=2, t=64)
            e4 = T5[:].rearrange("p (b two t) -> p b two t", two=2, t=64)
            P.op('pool', lambda: G.tensor_tensor(qhz[:, hh, :, 0, 0:64], q4[:, :, 0, :], e4[:, :, 0, :], ALU.mult), reads=[('qs', hh), K5],
                 writes=[('qhz', hh)])
            P.op('pool', lambda: G.tensor_tensor(qhz[:, hh, :, 1, 64:128], q4[:, :, 1, :], e4[:, :, 1, :], ALU.mult), reads=[('qs', hh), K5],
                 writes=[('qhz', hh)])
            P.op('dve', lambda: V.tensor_copy(dec[:, hh, :], hb3(T5[:])[:, :, 63]), reads=[K5], writes=[('dec', hh)])
            P.op('dve', lambda: V.tensor_tensor(hb3(T4[:]), b3, b3[:, :, 63:64].broadcast_to([128, NCH, 64]), ALU.subtract),
                 reads=[K3, K4], writes=[K4])
            P.op('act', lambda: A.activation(T5[:], T4[:], AF.Exp, scale=-1.0), reads=[K4, K5], writes=[K5])
            P.op('dve', lambda: V.tensor_tensor(kh[:, hh, :], T2[:], T5[:], ALU.mult), reads=[K2, K5], writes=[('kh', hh)])

        if stage < 5:
            continue
        for tb in range(NB):
            gblk = ti * NB + tb
            o_, okey = ob[gblk % 2], ('ob', gblk % 2)
            mi = 1 if gblk == 0 else 0
            for h in range(8):
                ch, base = h // 2, (h % 2) * 64
                r = h % 2
                sp_, sk = pb[(0, 2)[r]][:, 0:256], BK((0, 2)[r])
                P.op('pe', lambda: PE.matmul(sp_, qrot[base:base + 64, ch, tb * 128:(tb + 1) * 128],
                                             krot[base:base + 64, tb * 128:tb * 128 + 256], start=True, stop=False),
                     reads=[('qrot', ch), 'krot'], writes=[sk])
                P.op('pe', lambda: PE.matmul(sp_, identb[:], maskb[:, mi, :], start=False, stop=True), reads=['identb', 'maskb'], writes=[sk])
                s_ = st[r]
                stk = ('st', r)
                mx, negm, rsum, es, rden = (s_[:, i:i + 1] for i in range(5))
                P.op('dve', lambda: V.reduce_max(mx, sp_, AX.X), reads=[sk], writes=[stk])
                P.op('dve', lambda: V.tensor_scalar(negm, mx, -0.125, nsink[:, h:h + 1], ALU.mult, ALU.min), reads=[stk, 'nsink'], writes=[stk])
                p_, pkey = psb_[r], ('p', r)
                P.op('act', lambda: A.activation(p_[:], sp_, AF.Exp, bias=negm, scale=0.125, accum_out=rsum), reads=[sk, stk],
                     writes=[pkey, stk])
                P.op('act', lambda: A.activation(es, cst[:, 2 + h:3 + h], AF.Exp, bias=negm), reads=['cst', stk], writes=[stk])
                P.op('dve', lambda: V.tensor_tensor(rden, rsum, es, ALU.add), reads=[stk], writes=[stk])
                P.op('dve', lambda: V.reciprocal(rden, rden), reads=[stk], writes=[stk])
                tp, tk = pb[(1, 4)[r]][:, 0:256], BK((1, 4)[r])
                for half in range(2):
                    P.op('pe', lambda: PE.transpose(tp[:, half * 128:(half + 1) * 128], p_[:, half * 128:(half + 1) * 128], identb[:]),
                         reads=[pkey, 'identb'], writes=[tk])
                pt_, ptk = pTs[r], ('pT', r)
                P.op('act', lambda: A.copy(pt_[:].rearrange("p a b -> p (a b)"), tp), reads=[tk], writes=[ptk])
                op_, opk = pb[3][:, r * 64:(r + 1) * 64], BK(3)
                P.op('pe', lambda: PE.matmul(op_, pt_[:, 0, :], vA[:, tb, :], start=True, stop=False), reads=[ptk, ('vA', tb)], writes=[opk])
                P.op('pe', lambda: PE.matmul(op_, pt_[:, 1, :], vA[:, tb + 1, :], start=False, stop=True), reads=[ptk, ('vA', tb + 1)],
                     writes=[opk])
                P.op('dve', lambda: V.tensor_scalar(o_[:, h * 64:(h + 1) * 64], op_, rden, None, ALU.mult), reads=[opk, stk], writes=[okey])
            for hh in range(4 if stage >= 6 else 0):
                r = hh % 2
                j0, j1 = 2 * tb, 2 * tb + 1
                at, atk = pb[5][:, r * 128:(r + 1) * 128], BK(5)
                ktb = kt[:, hh, tb * 128:(tb + 1) * 128]
                P.op('pe', lambda: PE.matmul(at[:, 0:64], ktb, qt[:, hh, j0 * 64:(j0 + 1) * 64], start=True, stop=True),
                     reads=[('kt', hh), ('qt', hh)], writes=[atk])
                P.op('pe', lambda: PE.matmul(at[:, 64:128], ktb, qt[:, hh, j1 * 64:(j1 + 1) * 64], start=True, stop=True),
                     reads=[('kt', hh), ('qt', hh)], writes=[atk])
                am, amk = ATm[r], ('ATm', r)
                P.op('dve', lambda: V.tensor_tensor(am[0:64, 0:64], at[0:64, 0:64], tri[0:64, :], ALU.mult), reads=[atk, 'tri'], writes=[amk])
                P.op('dve', lambda: V.tensor_tensor(am[64:128, 64:128], at[64:128, 64:128], tri[64:128, :], ALU.mult), reads=[atk, 'tri'],
                     writes=[amk])
                ktp, ktk = pb[(1, 4)[r]][:, 256:384], BK((1, 4)[r])
                P.op('pe', lambda: PE.transpose(ktp, kh[:, hh, tb * 128:(tb + 1) * 128], identb[:]), reads=[('kh', hh), 'identb'], writes=[ktk])
                kT_, kTk = khT[r], ('khT', r)
                P.op('act', lambda: A.copy(kT_[:], ktp), reads=[ktk], writes=[kTk])
                ho, hok = pb[(6, 7)[r]][:, 0:128], BK((6, 7)[r])
                vblk = hv[:, tb, hh * 128:(hh + 1) * 128]
                P.op('pe', lambda: PE.matmul(ho, am[:], vblk, start=True, stop=False), reads=[amk, ('hv', tb, hh)], writes=[hok])
                par = 0
                P.op('pe', lambda: PE.matmul(ho, qhz[:, hh, tb, 0, :], Sb[:, hh, 0, :], start=False, stop=False),
                     reads=[('qhz', hh), ('Sb', hh, 0)], writes=[hok])
                for half, j in ((0, j0), (1, j1)):
                    kv, kvk = pb[5][:, 256 + half * 128:384 + half * 128], BK(5)
                    rows = slice(half * 64, half * 64 + 64)
                    P.op('pe', lambda: PE.matmul(kv, kT_[rows, :], hv[rows, tb, hh * 128:(hh + 1) * 128], start=True, stop=True),
                         reads=[kTk, ('hv', tb, hh)], writes=[kvk])
                    P.op('dve', lambda: V.scalar_tensor_tensor(S[:, hh, :], S[:, hh, :], dec[:, hh, j:j + 1], kv, ALU.mult, ALU.add),
                         reads=[('S', hh), ('dec', hh), kvk], writes=[('S', hh)])
                    sbp = 1 - half
                    P.op('act', lambda: A.copy(Sb[:, hh, sbp, :], S[:, hh, :]), reads=[('S', hh)], writes=[('Sb', hh, sbp)])
                    if half == 0:
                        P.op('pe', lambda: PE.matmul(ho, qhz[:, hh, tb, 1, :], Sb[:, hh, 1, :], start=False, stop=True),
                             reads=[('qhz', hh), ('Sb', hh, 1)], writes=[hok])
                s_ = st[r]
                stk = ('st', r)
                ss, rs = s_[:, 5:6], s_[:, 6:7]
                P.op('act', lambda: A.activation(junk[:], ho, AF.Square, accum_out=ss), reads=[hok], writes=['junk', stk])
                P.op('dve', lambda: V.tensor_scalar(rs, ss, 1.0 / 128, RMS_EPS, ALU.mult, ALU.add), reads=[stk], writes=[stk])
                P.op('act', lambda: A.activation(rs, rs, AF.Sqrt), reads=[stk], writes=[stk])
                P.op('dve', lambda: V.reciprocal(rs, rs), reads=[stk], writes=[stk])
                P.op('dve', lambda: V.scalar_tensor_tensor(o_[:, 512 + hh * 128:640 + hh * 128], ho, rs, ggw[:, tb, hh * 128:(hh + 1) * 128],
                                                           ALU.mult, ALU.mult), reads=[hok, stk, ('ggw', tb, hh)], writes=[okey])
            P.dma('sp', mo[t0 + tb * 128:t0 + (tb + 1) * 128, :], o_[:], reads=[okey], writes=[('mo', gblk)])
        P.op('act', lambda: A.copy(krot[:, 0:128], krot[:, TT:TT + 128]), reads=['krot'], writes=['krot'])
        P.op('act', lambda: A.copy(vA[:, 0, :], vA[:, NB, :]), reads=[('vA', NB)], writes=[('vA', 0)])
    P.finish([('mo', i) for i in range(NT * NB)])
    print("even: ins", P.n_ins, "waits", P.n_wait, "sems", P.nsem)
    ctx.close()
    return nc


def even_consts(TT=512):
    p = np.arange(128)
    pm = p % 64
    inv_freq = (ROPE_THETA ** (-np.arange(0, 16, 2, dtype=np.float32) / np.float32(16))).astype(np.float32)
    invf = np.where(pm < 16, inv_freq[pm % 8], 0.0).astype(np.float32)
    sgn = np.where(pm < 8, -1.0, np.where(pm < 16, 1.0, 0.0)).astype(np.float32)
    perm = np.zeros((128, 128), np.float32)
    for m in range(128):
        mm = m % 64
        if mm < 8:
            perm[m + 8, m] = 1.0
        elif mm < 16:
            perm[m - 8, m] = 1.0
    ident = np.eye(128, dtype=np.float32)
    q = np.arange(128)[:, None]
    k = np.arange(256)[None, :]
    allowed = (k > q) & (k <= q + 128)
    m0 = np.where(allowed, 0.0, -1e9).astype(np.float32)
    m1 = np.where(allowed & (k >= 128), 0.0, -1e9).astype(np.float32)
    masks = np.ascontiguousarray(np.stack([m0, m1], axis=1))
    s = np.arange(64)[:, None]
    t = np.arange(64)[None, :]
    tri64 = (s <= t).astype(np.float32)
    tri = np.concatenate([tri64, tri64], axis=0)
    smask = np.ones((128, TT), np.float32)
    smask[:, ::64] = 0.0
    return dict(invf=invf, sgn=sgn, perm=perm, identb=ident, masks=masks, tri=tri, smask=smask)


def even_inputs(h_b, pos_b, g, j, w_in, sinks, lb_logits, gnorm_w, consts, AW=2048, KV=256, HW=2048):
    D = h_b.shape[1]
    KC = D // 128
    cq = np.arange(512 * g, 512 * g + 512)
    ck = AW + 64 * g + np.arange(64)
    cv = AW + KV + 64 * g + np.arange(64)
    base = AW + 2 * KV
    chq = base + 512 * g + np.arange(512)
    chf = base + HW + 512 * g + np.arange(512)
    chi = base + 2 * HW + 512 * g + np.arange(512)
    chg = base + 3 * HW + 512 * g + np.arange(512)
    cols = np.concatenate([cq, ck, ck, chq, chf, chi, chg, cv, cv])
    wsl = w_in[:, cols]
    wl = np.ascontiguousarray(wsl.reshape(KC, 128, NBLK, 128).transpose(2, 1, 0, 3))
    cst = np.zeros((128, 16), np.float32)
    cst[:, 0] = consts["invf"]
    cst[:, 1] = consts["sgn"]
    cst[:, 2:10] = sinks[8 * g:8 * g + 8][None, :]
    cst[:, 10] = 1.0 if j == 1 else 0.0
    hsl = slice(512 * g, 512 * g + 512)
    l0 = lb_logits[0, hsl].reshape(4, 128).T
    l1 = lb_logits[1, hsl].reshape(4, 128).T
    lbl = np.ascontiguousarray(np.stack([l0, l1], axis=-1))
    gw = np.ascontiguousarray(np.broadcast_to(gnorm_w[None, :], (128, 128)))
    return {"hT": np.ascontiguousarray(h_b.T), "w": wl, "pos": np.ascontiguousarray(pos_b[None, :]).astype(np.int32), "cst": cst, "lbl": lbl,
            "gw": gw, "perm": consts["perm"], "identb": consts["identb"], "masks": consts["masks"], "tri": consts["tri"],
            "smask": consts["smask"]}

from concourse.bass_utils import run_bass_kernel_spmd

_PROGS = {}


def _prog(name, fn):
    if name not in _PROGS:
        _PROGS[name] = fn()
    return _PROGS[name]


def kernel(x, positions, even_w_in, even_w_out, attn_sinks, hgrn_lb_logits, hgrn_gnorm_w,
           rec_w_in, rec_conv_w, rec_conv_b, rec_gate_a_w, rec_gate_a_b, rec_gate_x_w, rec_gate_x_b,
           rec_lambda, rec_w_out, ln_mix_g, ln_mix_b, ln_ffn_g, ln_ffn_b,
           router_group_w, router_group_b, router_expert_w, router_expert_b,
           moe_w_gate, moe_w_up, moe_w_down):
    import ml_dtypes
    A_ = lambda a: np.asarray(a)
    x = A_(x).astype(np.float32)
    positions = A_(positions).astype(np.int32)
    B_, S_, D_ = x.shape
    DEPTH = A_(ln_mix_g).shape[0]
    NSL = 8 // B_
    TOK = S_ // NSL
    alpha = float((2.0 * DEPTH) ** 0.25)
    consts = even_consts()
    cores = list(range(8))
    h = x
    for layer in range(DEPTH):
        j = layer // 2
        mixed = np.empty((B_, S_, D_), dtype=ml_dtypes.bfloat16)
        if layer % 2 == 0:
            nc = _prog("even", lambda: build_even(D_, S_))
            w_in = A_(even_w_in[j])
            in_maps = [even_inputs(h[c // NSL], positions[c // NSL], c % NSL, j, w_in, A_(attn_sinks[j]), A_(hgrn_lb_logits),
                                   A_(hgrn_gnorm_w[j]), consts) for c in cores]
            res = run_bass_kernel_spmd(nc, in_maps, core_ids=cores)
            for c in cores:
                b, g = c // NSL, c % NSL
                mo = np.asarray(res.results[c]["mo"])
                mixed[b][:, 512 * g:512 * g + 512] = mo[:, 0:512]
                mixed[b][:, 2048 + 512 * g:2048 + 512 * g + 512] = mo[:, 512:1024]
            w_out = A_(even_w_out[j])
        else:
            nc = _prog("odd", lambda: build_odd(D_, S_, 4))
            in_maps = [odd_inputs(h[c // NSL], c % NSL, 4, A_(rec_w_in[j]), A_(rec_conv_w[j]), A_(rec_conv_b[j]), A_(rec_gate_a_w[j]),
                                  A_(rec_gate_a_b[j]), A_(rec_gate_x_w[j]), A_(rec_gate_x_b[j]), A_(rec_lambda[j])) for c in cores]
            res = run_bass_kernel_spmd(nc, in_maps, core_ids=cores)
            for c in cores:
                b, g = c // NSL, c % NSL
                mixed[b][:, 1024 * g:1024 * (g + 1)] = np.asarray(res.results[c]["oT"]).T
            w_out = A_(rec_w_out[j])
        del in_maps, res
        nc = _prog("b", lambda: build_b(D_, TOK, 4, 8, 384, alpha))
        W = b_weights(w_out, A_(ln_mix_g[layer]), A_(ln_mix_b[layer]), A_(ln_ffn_g[layer]), A_(ln_ffn_b[layer]),
                      A_(router_group_w[layer]), A_(router_group_b[layer]), A_(router_expert_w[layer]), A_(router_expert_b[layer]),
                      A_(moe_w_gate[layer]), A_(moe_w_up[layer]), A_(moe_w_down[layer]))
        in_maps = []
        for c in cores:
            b, s = c // NSL, c % NSL
            im = dict(W)
            im["mT"] = np.ascontiguousarray(mixed[b][s * TOK:(s + 1) * TOK].T)
            im["hT"] = np.ascontiguousarray(h[b][s * TOK:(s + 1) * TOK].T)
            in_maps.append(im)
        res = run_bass_kernel_spmd(nc, in_maps, core_ids=cores)
        hn = np.empty((B_, S_, D_), dtype=np.float32)
        for c in cores:
            b, s = c // NSL, c % NSL
            hn[b][s * TOK:(s + 1) * TOK] = np.asarray(res.results[c]["oT"]).T
        h = hn
        del in_maps, res, W
    return h
```

```python
from contextlib import ExitStack
import numpy as np
import concourse.bass as bass
import concourse.mybir as mybir

F32 = mybir.dt.float32
BF16 = mybir.dt.bfloat16
I32 = mybir.dt.int32
AF = mybir.ActivationFunctionType
ALU = mybir.AluOpType
AX = mybir.AxisListType

SEM_ROLL = 30000


class Prog:
    def __init__(self, nc, ctx, n_dma_sems=48):
        self.nc = nc
        self.ctx = ctx
        self.E = {'pe': nc.tensor, 'act': nc.scalar, 'dve': nc.vector, 'pool': nc.gpsimd, 'sp': nc.sync}
        self.cur_sem = {}
        self.cnt = {}
        self.nsem = 0
        for e in ('pe', 'act', 'dve', 'pool'):
            self._new_eng_sem(e)
        self.dq = {}
        for q, n in (('sp', 20), ('pool', 20), ('act', 6)):
            self.dq[q] = {'sems': [self._alloc_sem('d%s%d' % (q, i)) for i in range(n)], 'val': [0] * n, 'next': 0}
        self.known = {e: {} for e in self.E}
        self.lastw = {}
        self.readers = {}
        self.sem_by_id = {}
        self.n_ins = 0
        self.n_wait = 0

    def _alloc_sem(self, name):
        s = self.ctx.enter_context(self.nc.semaphore(name))
        self.nsem += 1
        return s

    def _new_eng_sem(self, e):
        self.cur_sem[e] = self._alloc_sem('s_%s_%d' % (e, self.nsem))
        self.cnt[e] = 0

    def _wait(self, e, tok):
        sem, val = tok
        k = self.known[e]
        sid = id(sem)
        if k.get(sid, 0) >= val:
            return
        self.E[e].wait_ge(sem, val)
        self.n_wait += 1
        k[sid] = val

    def _deps(self, e, reads, writes, pe_fifo=False):
        toks = []
        for key in reads:
            t = self.lastw.get(key)
            if t is not None:
                toks.append(t)
        for key in writes:
            t = self.lastw.get(key)
            if t is not None:
                toks.append(t)
            toks.extend(self.readers.get(key, ()))
        for t in toks:
            if pe_fifo and t[2] == 'pe':
                continue
            self._wait(e, t[:2])

    def _record(self, tok, reads, writes):
        for key in reads:
            self.readers.setdefault(key, []).append(tok)
        for key in writes:
            self.lastw[key] = tok
            self.readers[key] = []

    def op(self, e, fn, reads=(), writes=()):
        self._deps(e, reads, writes, pe_fifo=(e == 'pe'))
        ins = fn()
        if self.cnt[e] >= SEM_ROLL:
            self._new_eng_sem(e)
        self.cnt[e] += 1
        ins.then_inc(self.cur_sem[e], 1)
        tok = (self.cur_sem[e], self.cnt[e], e)
        self._record(tok, reads, writes)
        self.n_ins += 1
        return tok

    def _dma_common(self, q, issue, reads, writes):
        self._deps(q, reads, writes)
        d = self.dq[q]
        i = d['next']
        d['next'] = (i + 1) % len(d['sems'])
        if d['val'][i] > 0:
            self._wait(q, (d['sems'][i], d['val'][i]))
        if d['val'][i] >= SEM_ROLL:
            d['sems'][i] = self._alloc_sem('dr%d' % self.nsem)
            d['val'][i] = 0
        ins = issue()
        d['val'][i] += 16
        ins.then_inc(d['sems'][i], 16)
        tok = (d['sems'][i], d['val'][i], 'dma')
        self._record(tok, reads, writes)
        self.n_ins += 1
        return tok

    def dma(self, q, out, in_, reads=(), writes=(), **kw):
        return self._dma_common(q, lambda: self.E[q].dma_start(out=out, in_=in_, **kw), reads, writes)

    def raw_dma(self, q, fn, reads=(), writes=()):
        return self._dma_common(q, fn, reads, writes)

    def finish(self, keys):
        for key in keys:
            t = self.lastw.get(key)
            if t is not None:
                self._wait('sp', t[:2])
        for q, d in self.dq.items():
            for sem, val in zip(d['sems'], d['val']):
                if val > 0:
                    self._wait('sp', (sem, val))
        for sem, val in getattr(self, 'retired', []):
            self._wait('sp', (sem, val))

    def sb(self, name, shape, dt):
        return self.ctx.enter_context(self.nc.sbuf_tensor("sb_" + name, shape, dt))

    def ps(self, name, shape, dt=F32):
        return self.ctx.enter_context(self.nc.psum_tensor("pp_" + name, shape, dt))


RG_C = 8.0
GELU_C = 0.7978845608028654


def build_odd(D, T, NH, TT=512):
    KC = D // 128
    CC = NH * 2
    NOC = 2 * CC
    NT = T // TT
    nc = bass.Bass("TRN2", target_bir_lowering=False)
    hT = nc.dram_tensor("hT", [D, T], F32, kind="ExternalInput").ap()
    w = nc.dram_tensor("w", [NOC, 128, KC, 128], F32, kind="ExternalInput").ap()
    vec = nc.dram_tensor("vec", [128, CC, 8], F32, kind="ExternalInput").ap()
    wg = nc.dram_tensor("wg", [128, 2, NH, 2, 256], F32, kind="ExternalInput").ap()
    oT = nc.dram_tensor("oT", [CC * 128, T], BF16, kind="ExternalOutput").ap()
    hT_v = hT.rearrange("(c p) t -> p c t", p=128)
    oT_v = oT.rearrange("(c p) t -> p c t", p=128)
    ctx = ExitStack()
    P = Prog(nc, ctx)
    V, A, G = nc.vector, nc.scalar, nc.gpsimd
    NWB = 3
    hs = [P.sb("hs%d" % i, [128, KC, TT], BF16) for i in range(2)]
    wb = [P.sb("wb%d" % i, [128, KC, 128], BF16) for i in range(NWB)]
    vs = P.sb("vs", [128, CC, 8], F32)
    sc = P.sb("sc", [128, CC, 2], F32)
    wgs = P.sb("wgs", [128, 2, NH, 2, 256], BF16)
    gate = P.sb("gate", [128, CC, TT], F32)
    xb = P.sb("xb", [128, CC, TT + 3], F32)
    cv = P.sb("cv", [128, CC, TT], F32)
    cvb = P.sb("cvb", [128, CC, TT], BF16)
    hst = P.sb("hst", [128, CC, 2], F32)
    tmp = [P.sb("tmp%d" % i, [128, 6, TT], F32) for i in range(2)]
    ob = [P.sb("ob%d" % i, [128, TT], BF16) for i in range(2)]
    pss = [P.ps("ps%d" % i, [128, 512]) for i in range(8)]

    P.dma('sp', vs[:], vec, writes=['vs'])
    P.dma('pool', wgs[:], wg, writes=['wgs'])
    P.op('act', lambda: A.activation(sc[:, :, 1], vs[:, :, 7], AF.Exp, scale=-1.0), reads=['vs'], writes=['sc1'])
    P.op('act', lambda: A.activation(sc[:, :, 0], sc[:, :, 1], AF.Ln, bias=1.0), reads=['sc1'], writes=['sc0'])
    P.op('dve', lambda: V.tensor_scalar_mul(sc[:, :, 0], sc[:, :, 0], -RG_C), reads=['sc0'], writes=['sc0'])
    P.op('dve', lambda: V.memset(xb[:, :, 0:3], 0.0), writes=[('xb', c) for c in range(CC)])
    P.op('dve', lambda: V.memset(hst[:], 0.0), writes=['hst'])

    wcnt = 0
    pcnt = 0
    for ti in range(NT):
        t0 = ti * TT
        hsb = hs[ti % 2]
        P.dma('pool', hsb[:], hT_v[:, :, t0:t0 + TT], writes=[('hs', ti % 2)])
        for oc in range(NOC):
            wbi = wcnt % NWB
            wcnt += 1
            P.dma('pool', wb[wbi][:], w[oc], writes=[('wb', wbi)])
            ps = pss[pcnt % 2]
            pk = ('ps', pcnt % 2)
            pcnt += 1
            for c in range(KC):
                P.op('pe', lambda: nc.tensor.matmul(ps[:], wb[wbi][:, c, :], hsb[:, c, :], start=(c == 0), stop=(c == KC - 1)),
                     reads=[('wb', wbi), ('hs', ti % 2)], writes=[pk])
            if oc < CC:
                cc = oc
                tm = tmp[cc % 2]
                tk = ('tmp', cc % 2)
                P.op('act', lambda: A.activation(tm[:, 0, :], ps[:], AF.Square), reads=[pk], writes=[tk])
                P.op('dve', lambda: V.tensor_scalar(tm[:, 0, :], tm[:, 0, :], 2 * GELU_C * 0.044715, 2 * GELU_C, ALU.mult, ALU.add),
                     reads=[tk], writes=[tk])
                P.op('dve', lambda: V.tensor_tensor(tm[:, 0, :], tm[:, 0, :], ps[:], ALU.mult), reads=[tk, pk], writes=[tk])
                P.op('act', lambda: A.activation(tm[:, 0, :], tm[:, 0, :], AF.Sigmoid), reads=[tk], writes=[tk])
                P.op('dve', lambda: V.tensor_tensor(gate[:, cc, :], tm[:, 0, :], ps[:], ALU.mult), reads=[tk, pk],
                     writes=[('gate', cc)])
            else:
                cc = oc - CC
                P.op('act', lambda: A.copy(xb[:, cc, 3:3 + TT], ps[:]), reads=[pk], writes=[('xb', cc)])
                P.op('dve', lambda: V.tensor_scalar(cv[:, cc, :], xb[:, cc, 0:TT], vs[:, cc, 0:1], vs[:, cc, 4:5], ALU.mult, ALU.add),
                     reads=[('xb', cc), 'vs'], writes=[('cv', cc)])
                for j in range(1, 4):
                    P.op('dve', lambda: V.scalar_tensor_tensor(cv[:, cc, :], xb[:, cc, j:j + TT], vs[:, cc, j:j + 1], cv[:, cc, :],
                                                               ALU.mult, ALU.add),
                         reads=[('xb', cc), 'vs', ('cv', cc)], writes=[('cv', cc)])
                P.op('act', lambda: A.copy(cvb[:, cc, :], cv[:, cc, :]), reads=[('cv', cc)], writes=[('cvb', cc)])
                P.op('pool', lambda: G.tensor_copy(xb[:, cc, 0:3], xb[:, cc, TT:TT + 3]), reads=[('xb', cc)], writes=[('xb', cc)])
        for cc in range(CC):
            hh, jc = cc // 2, cc % 2
            tm = tmp[cc % 2]
            tk = ('tmp', cc % 2)
            psa = pss[2 + (cc % 2) * 2]
            psx = pss[3 + (cc % 2) * 2]
            ka, kx = ('ps', 2 + (cc % 2) * 2), ('ps', 3 + (cc % 2) * 2)
            for gi, (pg, kg) in enumerate(((psa, ka), (psx, kx))):
                for ic in range(2):
                    P.op('pe', lambda: nc.tensor.matmul(pg[:], wgs[:, gi, hh, ic, jc * 128:(jc + 1) * 128], cvb[:, 2 * hh + ic, :],
                                                        start=(ic == 0), stop=(ic == 1)),
                         reads=['wgs', ('cvb', 2 * hh + ic)], writes=[kg])
            r, ig, a, a2, bb, hcur = (tm[:, i, :] for i in range(6))
            P.op('act', lambda: A.activation(r, psa[:], AF.Sigmoid, bias=vs[:, cc, 5:6]), reads=[ka, 'vs'], writes=[tk])
            P.op('act', lambda: A.activation(ig, psx[:], AF.Sigmoid, bias=vs[:, cc, 6:7]), reads=[kx, 'vs'], writes=[tk])
            P.op('act', lambda: A.activation(a, r, AF.Exp, scale=sc[:, cc, 0:1]), reads=[tk, 'sc0'], writes=[tk])
            P.op('dve', lambda: V.tensor_tensor(a2, a, a, ALU.mult), reads=[tk], writes=[tk])
            P.op('dve', lambda: V.tensor_scalar(a2, a2, -1.0, 1.0, ALU.mult, ALU.add), reads=[tk], writes=[tk])
            P.op('dve', lambda: V.tensor_scalar_max(a2, a2, 0.0), reads=[tk], writes=[tk])
            P.op('act', lambda: A.activation(a2, a2, AF.Sqrt), reads=[tk], writes=[tk])
            P.op('pool', lambda: G.tensor_tensor(bb, ig, cv[:, cc, :], ALU.mult), reads=[tk, ('cv', cc)], writes=[tk])
            P.op('dve', lambda: V.tensor_tensor(bb, bb, a2, ALU.mult), reads=[tk], writes=[tk])
            P.op('dve', lambda: V.tensor_tensor_scan(hcur, a, bb, hst[:, cc, (ti % 2):(ti % 2) + 1], ALU.mult, ALU.add),
                 reads=[tk, ('hst', cc, ti % 2)], writes=[tk])
            P.op('act', lambda: A.copy(hst[:, cc, ((ti + 1) % 2):((ti + 1) % 2) + 1], hcur[:, TT - 1:TT]), reads=[tk],
                 writes=[('hst', cc, (ti + 1) % 2)])
            obb = ob[cc % 2]
            P.op('dve', lambda: V.tensor_tensor(obb[:], hcur, gate[:, cc, :], ALU.mult), reads=[tk, ('gate', cc)],
                 writes=[('ob', cc % 2)])
            P.dma('sp', oT_v[:, cc, t0:t0 + TT], obb[:], reads=[('ob', cc % 2)], writes=[('oT', cc, ti)])
    P.finish([('oT', cc, ti) for cc in range(CC) for ti in range(NT)])
    print("odd: ins", P.n_ins, "waits", P.n_wait, "sems", P.nsem)
    ctx.close()
    return nc


def odd_inputs(h_b, g, NH, rec_w_in, conv_w, conv_b, wa, ba, wx, bx, lam):
    D = h_b.shape[1]
    LRU = conv_w.shape[1]
    C = NH * 256
    CC = C // 128
    KC = D // 128
    cols = np.concatenate([np.arange(g * C, (g + 1) * C), LRU + np.arange(g * C, (g + 1) * C)])
    wsl = rec_w_in[:, cols]
    wl = np.ascontiguousarray(wsl.reshape(KC, 128, 2 * CC, 128).transpose(2, 1, 0, 3))
    sl = slice(g * C, (g + 1) * C)
    vecs = np.stack([conv_w[0, sl], conv_w[1, sl], conv_w[2, sl], conv_w[3, sl], conv_b[sl], ba[sl], bx[sl], lam[sl]], axis=-1)
    vecs = np.ascontiguousarray(vecs.reshape(CC, 128, 8).transpose(1, 0, 2))
    wgl = np.stack([wa[g * NH:(g + 1) * NH], wx[g * NH:(g + 1) * NH]], axis=0)
    wgl = np.ascontiguousarray(wgl.reshape(2, NH, 2, 128, 256).transpose(3, 0, 1, 2, 4))
    return {"hT": np.ascontiguousarray(h_b.T), "w": wl, "vec": vecs, "wg": wgl}


LN_EPS = 1e-5


def ln_feature_major(P, nc, buf, KC, TT, ones, gb, gi, bi, pss, pskeys, sq, stat, out_fn):
    V, A, G = nc.vector, nc.scalar, nc.gpsimd
    ps1, ps2 = pss
    k1, k2 = pskeys
    for c in range(KC):
        P.op('pe', lambda: nc.tensor.matmul(ps1[:], ones[:], buf[:, c, :], start=(c == 0), stop=(c == KC - 1)),
             reads=[('buf', c), 'ones'], writes=[k1])
    for c in range(KC):
        s = sq[c % 2]
        P.op('act', lambda: A.activation(s[:], buf[:, c, :], AF.Square), reads=[('buf', c)], writes=[('sq', c % 2)])
        P.op('pe', lambda: nc.tensor.matmul(ps2[:], ones[:], s[:], start=(c == 0), stop=(c == KC - 1)),
             reads=[('sq', c % 2), 'ones'], writes=[k2])
    mean, rstd, nmr = stat[:, 0, :], stat[:, 1, :], stat[:, 2, :]
    P.op('act', lambda: A.copy(mean, ps1[:]), reads=[k1], writes=['stat'])
    P.op('dve', lambda: V.tensor_tensor(nmr, mean, mean, ALU.mult), reads=['stat'], writes=['stat'])
    P.op('dve', lambda: V.tensor_tensor(rstd, ps2[:], nmr, ALU.subtract), reads=[k2, 'stat'], writes=['stat'])
    P.op('dve', lambda: V.tensor_scalar(rstd, rstd, 0.0, LN_EPS, ALU.max, ALU.add), reads=['stat'], writes=['stat'])
    P.op('act', lambda: A.activation(rstd, rstd, AF.Sqrt), reads=['stat'], writes=['stat'])
    P.op('dve', lambda: V.reciprocal(rstd, rstd), reads=['stat'], writes=['stat'])
    P.op('dve', lambda: V.scalar_tensor_tensor(nmr, mean, -1.0, rstd, ALU.mult, ALU.mult), reads=['stat'], writes=['stat'])
    for c in range(KC):
        s = sq[c % 2]
        P.op('pool', lambda: G.tensor_tensor(s[:], buf[:, c, :], rstd, ALU.mult), reads=[('buf', c), 'stat'], writes=[('sq', c % 2)])
        P.op('dve', lambda: V.tensor_tensor(s[:], s[:], nmr, ALU.add), reads=[('sq', c % 2), 'stat'], writes=[('sq', c % 2)])
        out_fn(c, s, ('sq', c % 2))


def build_b(D, NTOK, NG, EPG, FF, alpha, TT=512):
    KC = D // 128
    E = NG * EPG
    FC = FF // 128
    NT = NTOK // TT
    NB = TT // 128
    NR = NG + E
    nc = bass.Bass("TRN2", target_bir_lowering=False)
    mT = nc.dram_tensor("mT", [D, NTOK], BF16, kind="ExternalInput").ap()
    hT = nc.dram_tensor("hT", [D, NTOK], F32, kind="ExternalInput").ap()
    wo = nc.dram_tensor("wo", [KC, 128, KC, 128], F32, kind="ExternalInput").ap()
    gbd = nc.dram_tensor("gb", [128, KC, 4], F32, kind="ExternalInput").ap()
    wrd = nc.dram_tensor("wr", [128, KC, NR], F32, kind="ExternalInput").ap()
    brd = nc.dram_tensor("br", [128, NR], F32, kind="ExternalInput").ap()
    wgu = nc.dram_tensor("wgu", [E, 2, FC, 128, KC, 128], F32, kind="ExternalInput").ap()
    wd = nc.dram_tensor("wd", [E, FC * 128, D], F32, kind="ExternalInput").ap()
    identd = nc.dram_tensor("ident", [128, 128], F32, kind="ExternalInput").ap()
    seld = nc.dram_tensor("sel", [E, E, 128], F32, kind="ExternalInput").ap()
    oT = nc.dram_tensor("oT", [D, NTOK], F32, kind="ExternalOutput").ap()
    mT_v = mT.rearrange("(c p) t -> p c t", p=128)
    hT_v = hT.rearrange("(c p) t -> p c t", p=128)
    oT_v = oT.rearrange("(c p) t -> p c t", p=128)
    ctx = ExitStack()
    P = Prog(nc, ctx)
    V, A, G = nc.vector, nc.scalar, nc.gpsimd
    NWB = 4
    bufA = P.sb("bufA", [128, KC, TT], BF16)
    buf = P.sb("buf", [128, KC, TT], F32)
    wb = [P.sb("wb%d" % i, [128, KC, 128], BF16) for i in range(NWB)]
    wdb = [P.sb("wdb%d" % i, [128, FC, D], BF16) for i in range(2)]
    sq = [P.sb("sq%d" % i, [128, TT], F32) for i in range(2)]
    stat = P.sb("stat", [128, 3, TT], F32)
    gb = P.sb("gbs", [128, KC, 4], F32)
    wr = P.sb("wrs", [128, KC, NR], F32)
    br = P.sb("brs", [128, NR], F32)
    ident = P.sb("idents", [128, 128], F32)
    ones = P.sb("ones", [128, 128], F32)
    selb = [P.sb("selb%d" % i, [E, 128], F32) for i in range(2)]
    rt = P.sb("rt", [128, 16, NR], F32)
    cw = P.sb("cw", [128, E], F32)
    cwT = P.sb("cwT", [E, TT], F32)
    cwb = [P.sb("cwb0", [128, TT], F32)] * 2
    sg = [P.sb("sg%d" % i, [128, TT], F32) for i in range(2)]
    hid = [P.sb("hid0", [128, FC, TT], BF16)] * 2
    pss = [P.ps("ps%d" % i, [128, 512]) for i in range(8)]

    P.dma('sp', gb[:], gbd, writes=['gb'])
    P.dma('sp', wr[:], wrd, writes=['wr'])
    P.dma('sp', br[:], brd, writes=['br'])
    P.dma('sp', ident[:], identd, writes=['ident'])
    P.op('dve', lambda: V.memset(ones[:], 1.0 / D), writes=['ones'])

    wcnt = [0]

    def load_w(src):
        i = wcnt[0] % NWB
        wcnt[0] += 1
        P.dma('pool', wb[i][:], src, writes=[('wb', i)])
        return i

    hcnt = 0
    ocnt = 0
    wdcnt = 0
    for ti in range(NT):
        t0 = ti * TT
        P.dma('pool', bufA[:], mT_v[:, :, t0:t0 + TT], writes=[('bufA', c) for c in range(KC)])
        for oc in range(KC):
            wi = load_w(wo[oc])
            hb = sg[hcnt % 2]
            hk = ('sg', hcnt % 2)
            hcnt += 1
            P.dma('sp', hb[:], hT_v[:, oc, t0:t0 + TT], writes=[hk])
            ps = pss[oc % 2]
            pk = ('ps', oc % 2)
            for c in range(KC):
                P.op('pe', lambda: nc.tensor.matmul(ps[:], wb[wi][:, c, :], bufA[:, c, :], start=(c == 0), stop=(c == KC - 1)),
                     reads=[('wb', wi), ('bufA', c)], writes=[pk])
            P.op('dve', lambda: V.scalar_tensor_tensor(buf[:, oc, :], hb[:], alpha, ps[:], ALU.mult, ALU.add),
                 reads=[hk, pk], writes=[('buf', oc)])

        def out1(c, s, sk):
            P.op('act', lambda: A.activation(bufA[:, c, :], s[:], AF.Identity, bias=gb[:, c, 1:2], scale=gb[:, c, 0:1]),
                 reads=[sk, 'gb'], writes=[('bufA', c)])
            P.op('dve', lambda: V.tensor_scalar(s[:], s[:], gb[:, c, 0:1], gb[:, c, 1:2], ALU.mult, ALU.add),
                 reads=[sk, 'gb'], writes=[sk])
            P.op('pool', lambda: G.tensor_scalar(buf[:, c, :], s[:], alpha, None, ALU.mult), reads=[sk], writes=[('buf', c)])

        ln_feature_major(P, nc, buf, KC, TT, ones, gb, 0, 1, (pss[6], pss[7]), (('ps', 6), ('ps', 7)), sq, stat, out1)

        for tb in range(NB):
            psr = pss[6]
            kr = ('ps', 6)
            for c in range(KC):
                P.op('pe', lambda: nc.tensor.matmul(psr[:, 0:NR], buf[:, c, tb * 128:(tb + 1) * 128], wr[:, c, :],
                                                    start=(c == 0), stop=(c == KC - 1)),
                     reads=[('buf', c), 'wr'], writes=[kr])
            R_ = lambda i, n: rt[:, i, 0:n]
            lg = rt[:, 0, :]
            K = ['rt']
            dv = lambda fn, extra=(): P.op('dve', fn, reads=K + list(extra), writes=K)
            dv(lambda: V.scalar_tensor_tensor(lg, psr[:, 0:NR], 1.0 / alpha, br[:], ALU.mult, ALU.add), [kr, 'br'])
            gl = rt[:, 0, 0:NG]
            el = rt[:, 0, NG:NR]
            gmax, ngmax, gsum, ptop = (rt[:, 1, i:i + 1] for i in range(4))
            m1, m2, dd, w1, w2 = (rt[:, 1, i:i + 1] for i in range(4, 9))
            og = R_(2, NG)
            ge = R_(3, NG)
            e8, o1, e8b, o2, c8 = (R_(i, EPG) for i in range(4, 9))
            dv(lambda: V.reduce_max(gmax, gl, AX.X))
            dv(lambda: V.tensor_scalar(ngmax, gmax, -1.0, None, ALU.mult))
            P.op('act', lambda: A.activation(ge, gl, AF.Exp, bias=ngmax, accum_out=gsum), reads=K, writes=K)
            dv(lambda: V.reciprocal(ptop, gsum))
            dv(lambda: V.tensor_scalar(og, gl, gmax, None, ALU.is_equal))
            dv(lambda: V.tensor_scalar(e8, el[:, 0:EPG], og[:, 0:1], None, ALU.mult))
            for g in range(1, NG):
                dv(lambda: V.scalar_tensor_tensor(e8, el[:, g * EPG:(g + 1) * EPG], og[:, g:g + 1], e8, ALU.mult, ALU.add))
            dv(lambda: V.reduce_max(m1, e8, AX.X))
            dv(lambda: V.tensor_scalar(o1, e8, m1, None, ALU.is_equal))
            dv(lambda: V.scalar_tensor_tensor(e8b, o1, -1e30, e8, ALU.mult, ALU.add))
            dv(lambda: V.reduce_max(m2, e8b, AX.X))
            dv(lambda: V.tensor_scalar(o2, e8b, m2, None, ALU.is_equal))
            dv(lambda: V.tensor_tensor(dd, m2, m1, ALU.subtract))
            P.op('act', lambda: A.activation(dd, dd, AF.Exp), reads=K, writes=K)
            dv(lambda: V.tensor_scalar(w1, dd, 1.0, None, ALU.add))
            dv(lambda: V.reciprocal(w1, w1))
            dv(lambda: V.tensor_tensor(w2, dd, w1, ALU.mult))
            dv(lambda: V.tensor_tensor(w1, w1, ptop, ALU.mult))
            dv(lambda: V.tensor_tensor(w2, w2, ptop, ALU.mult))
            dv(lambda: V.tensor_scalar(c8, o1, w1, None, ALU.mult))
            dv(lambda: V.scalar_tensor_tensor(c8, o2, w2, c8, ALU.mult, ALU.add))
            for g in range(NG):
                P.op('dve', lambda: V.tensor_scalar(cw[:, g * EPG:(g + 1) * EPG], c8, og[:, g:g + 1], None, ALU.mult),
                     reads=K, writes=['cw'])
            pst = pss[7]
            kt = ('ps', 7)
            P.op('pe', lambda: nc.tensor.transpose(pst[0:E, 0:128], cw[:], ident[:]), reads=['cw', 'ident'], writes=[kt])
            P.op('act', lambda: A.copy(cwT[:, tb * 128:(tb + 1) * 128], pst[0:E, 0:128]), reads=[kt], writes=['cwT'])

        reqs = []
        for e in range(E):
            for fc in range(FC):
                reqs.append(('gu', e, fc))
                if fc == min(1, FC - 1):
                    reqs.append(('wd', e, 0))
        rpos = {r: i for i, r in enumerate(reqs)}
        slots = {}
        rptr = [0]

        def issue_upto(n):
            nonlocal wdcnt
            while rptr[0] < min(n, len(reqs)):
                kind, e_, fc_ = reqs[rptr[0]]
                if kind == 'gu':
                    slots[(kind, e_, fc_)] = (load_w(wgu[e_, 0, fc_]), load_w(wgu[e_, 1, fc_]))
                else:
                    wdi_ = wdcnt % 2
                    wdcnt += 1
                    P.dma('pool', wdb[wdi_][:], wd[e_].rearrange("(s p) d -> p s d", p=128), writes=[('wdb', wdi_)])
                    slots[(kind, e_, 0)] = wdi_
                rptr[0] += 1

        gu_issued = [0]

        def issue_gu_ahead(k):
            while rptr[0] < len(reqs):
                kind = reqs[rptr[0]][0]
                if kind == 'gu' and gu_issued[0] >= k + 2:
                    break
                if kind == 'gu':
                    gu_issued[0] += 1
                issue_upto(rptr[0] + 1)

        for e in range(E):
            psb = pss[6]
            sb_ = selb[e % 2]
            P.dma('sp', sb_[:], seld[:, e, :], writes=[('selb', e % 2)])
            P.op('pe', lambda: nc.tensor.matmul(psb[:], sb_[:], cwT[:], start=True, stop=True),
                 reads=[('selb', e % 2), 'cwT'], writes=[('ps', 6)])
            cb = cwb[e % 2]
            P.op('act', lambda: A.copy(cb[:], psb[:]), reads=[('ps', 6)], writes=[('cwb', 0)])
            hd = hid[e % 2]
            for fc in range(FC):
                issue_gu_ahead(e * FC + fc)
                wgi, wui = slots[('gu', e, fc)]
                pg, pu = pss[fc % 2], pss[2 + fc % 2]
                kg, ku = ('ps', fc % 2), ('ps', 2 + fc % 2)
                for c in range(KC):
                    P.op('pe', lambda: nc.tensor.matmul(pg[:], wb[wgi][:, c, :], bufA[:, c, :], start=(c == 0), stop=(c == KC - 1)),
                         reads=[('wb', wgi), ('bufA', c)], writes=[kg])
                for c in range(KC):
                    P.op('pe', lambda: nc.tensor.matmul(pu[:], wb[wui][:, c, :], bufA[:, c, :], start=(c == 0), stop=(c == KC - 1)),
                         reads=[('wb', wui), ('bufA', c)], writes=[ku])
                s_ = sg[fc % 2]
                sk = ('sg', fc % 2)
                P.op('act', lambda: A.activation(s_[:], pg[:], AF.Silu), reads=[kg], writes=[sk])
                P.op('dve', lambda: V.tensor_tensor(s_[:], s_[:], pu[:], ALU.mult), reads=[sk, ku], writes=[sk])
                P.op('dve', lambda: V.tensor_tensor(hd[:, fc, :], s_[:], cb[:], ALU.mult), reads=[sk, ('cwb', 0)],
                     writes=[('hid', 0, fc)])
            issue_upto(rpos[('wd', e, 0)] + 1)
            wdi = slots[('wd', e, 0)]
            for dc in range(KC):
                py = pss[4 + dc % 2]
                ky = ('ps', 4 + dc % 2)
                for fc in range(FC):
                    P.op('pe', lambda: nc.tensor.matmul(py[:], wdb[wdi][:, fc, dc * 128:(dc + 1) * 128], hd[:, fc, :],
                                                        start=(fc == 0), stop=(fc == FC - 1)),
                         reads=[('wdb', wdi), ('hid', 0, fc)], writes=[ky])
                P.op('dve', lambda: V.tensor_tensor(buf[:, dc, :], buf[:, dc, :], py[:], ALU.add), reads=[('buf', dc), ky],
                     writes=[('buf', dc)])

        def out2(c, s, sk):
            nonlocal ocnt
            ob = sg[ocnt % 2]
            ok = ('sg', ocnt % 2)
            ocnt += 1
            P.op('act', lambda: A.activation(ob[:], s[:], AF.Identity, bias=gb[:, c, 3:4], scale=gb[:, c, 2:3]),
                 reads=[sk, 'gb'], writes=[ok])
            P.dma('sp', oT_v[:, c, t0:t0 + TT], ob[:], reads=[ok], writes=[('oT', c, ti)])

        ln_feature_major(P, nc, buf, KC, TT, ones, gb, 2, 3, (pss[6], pss[7]), (('ps', 6), ('ps', 7)), sq, stat, out2)
    P.finish([('oT', c, ti) for c in range(KC) for ti in range(NT)])
    print("B: ins", P.n_ins, "waits", P.n_wait, "sems", P.nsem)
    ctx.close()
    return nc


def b_consts(E):
    ident = np.eye(128, dtype=np.float32)
    sel = np.zeros((E, E, 128), np.float32)
    for e in range(E):
        sel[e, e, :] = 1.0
    return ident, sel


def b_weights(w_out, ln_mix_g, ln_mix_b, ln_ffn_g, ln_ffn_b, wg_r, bg_r, we_r, be_r, w_gate, w_up, w_down):
    D = w_out.shape[1]
    KC = D // 128
    E, _, FF = w_gate.shape
    FC = FF // 128
    wo = np.ascontiguousarray(w_out.reshape(KC, 128, KC, 128).transpose(2, 1, 0, 3))
    gb = np.stack([ln_mix_g, ln_mix_b, ln_ffn_g, ln_ffn_b], axis=-1)
    gb = np.ascontiguousarray(gb.reshape(KC, 128, 4).transpose(1, 0, 2))
    wr = np.concatenate([wg_r, we_r], axis=1)
    NR = wr.shape[1]
    wr = np.ascontiguousarray(wr.reshape(KC, 128, NR).transpose(1, 0, 2))
    br = np.ascontiguousarray(np.broadcast_to(np.concatenate([bg_r, be_r])[None, :], (128, NR)))
    wgu = np.stack([w_gate, w_up], axis=1)
    wgu = np.ascontiguousarray(wgu.reshape(E, 2, KC, 128, FC, 128).transpose(0, 1, 4, 3, 2, 5))
    ident, sel = b_consts(E)
    return {"wo": wo, "gb": gb, "wr": wr, "br": br, "wgu": wgu, "wd": np.ascontiguousarray(w_down), "ident": ident, "sel": sel}

import math

RMS_EPS = 1e-6
ROPE_THETA = 500000.0
NBLK = 22


def build_even(D, T, TT=512, stage=9):
    KC = D // 128
    NT = T // TT
    NB = TT // 128
    NCH = TT // 64
    nc = bass.Bass("TRN2", target_bir_lowering=False)
    dt_in = lambda n, s, d=F32: nc.dram_tensor(n, s, d, kind="ExternalInput").ap()
    hT = dt_in("hT", [D, T])
    w = dt_in("w", [NBLK, 128, KC, 128])
    pos = dt_in("pos", [1, T], I32)
    cstd = dt_in("cst", [128, 16])
    lbld = dt_in("lbl", [128, 4, 2])
    gwd = dt_in("gw", [128, 128])
    permd = dt_in("perm", [128, 128])
    identd = dt_in("identb", [128, 128])
    maskd = dt_in("masks", [128, 2, 256])
    trid = dt_in("tri", [128, 64])
    smaskd = dt_in("smask", [128, TT])
    mo = nc.dram_tensor("mo", [T, 1024], BF16, kind="ExternalOutput").ap()
    hT_v = hT.rearrange("(c p) t -> p c t", p=128)
    ctx = ExitStack()
    P = Prog(nc, ctx)
    V, A, G, PE = nc.vector, nc.scalar, nc.gpsimd, nc.tensor
    NWB = 3
    hs = P.sb("hs", [128, KC, TT], BF16)
    wb = [P.sb("wb%d" % i, [128, KC, 128], BF16) for i in range(NWB)]
    cst = P.sb("cst", [128, 16], F32)
    lbl = P.sb("lbl", [128, 4, 2], F32)
    lbv = P.sb("lbv", [128, 4, 4], F32)
    nsink = P.sb("nsink", [128, 8], F32)
    gwb = P.sb("gwb", [128, 128], F32)
    perm = P.sb("perm", [128, 128], F32)
    identb = P.sb("identb", [128, 128], BF16)
    maskb = P.sb("maskb", [128, 2, 256], BF16)
    tri = P.sb("tri", [128, 64], F32)
    smask = P.sb("smask", [128, TT], F32)
    posi = P.sb("posi", [128, TT], I32)
    cosF = P.sb("cosF", [128, TT], F32)
    sinF = P.sb("sinF", [128, TT], F32)
    tmp = [P.sb("tmp%d" % i, [128, TT], F32) for i in range(10)]
    qf = [P.sb("qf%d" % i, [128, TT], F32) for i in range(2)]
    qrot = P.sb("qrot", [128, 4, TT], BF16)
    krot = P.sb("krot", [128, 128 + TT], BF16)
    vA = P.sb("vA", [128, NB + 1, 64], BF16)
    hv = P.sb("hv", [128, NB, 512], BF16)
    ggw = P.sb("ggw", [128, NB, 512], F32)
    gs = [P.sb("gs%d" % i, [128, 128], F32) for i in range(2)]
    qs = P.sb("qs", [128, 4, TT], F32)
    sig = P.sb("sig", [128, 4, TT], F32)
    qt = P.sb("qt", [128, 4, TT], BF16)
    kt = P.sb("kt", [128, 4, TT], BF16)
    kh = P.sb("kh", [128, 4, TT], BF16)
    qhz = P.sb("qhz", [128, 4, NB, 2, 128], BF16)
    dec = P.sb("dec", [128, 4, NCH], F32)
    khT = [P.sb("khT%d" % i, [128, 128], BF16) for i in range(2)]
    S = P.sb("S", [128, 4, 128], F32)
    Sb = P.sb("Sb", [128, 4, 2, 128], BF16)
    ATm = [P.sb("ATm%d" % i, [128, 128], BF16) for i in range(2)]
    psb_ = [P.sb("p%d" % i, [128, 256], BF16) for i in range(2)]
    pTs = [P.sb("pT%d" % i, [128, 2, 128], BF16) for i in range(2)]
    st = [P.sb("st%d" % i, [128, 8], F32) for i in range(2)]
    junk = P.sb("junk", [128, 128], F32)
    ob = [P.sb("ob%d" % i, [128, 1024], BF16) for i in range(2)]
    pb = [P.ps("pb%d" % i, [128, 1024], BF16) if i in (1, 4) else P.ps("pb%d" % i, [128, 512]) for i in range(8)]
    BK = lambda i: ('pb', i)

    P.dma('sp', cst[:], cstd, writes=['cst'])
    P.dma('sp', lbl[:], lbld, writes=['lbl'])
    P.dma('sp', gwb[:], gwd, writes=['gwb'])
    P.dma('sp', perm[:], permd, writes=['perm'])
    P.dma('sp', tri[:], trid, writes=['tri'])
    P.dma('sp', smask[:], smaskd, writes=['smask'])
    P.dma('pool', identb[:], identd, writes=['identb'])
    P.dma('pool', maskb[:], maskd, writes=['maskb'])
    lb, oml, noml, ltmp = (lbv[:, :, i] for i in range(4))
    P.op('dve', lambda: V.tensor_tensor(ltmp, lbl[:, :, 1], lbl[:, :, 0], ALU.subtract), reads=['lbl'], writes=['lbv'])
    P.op('act', lambda: A.activation(lb, ltmp, AF.Sigmoid), reads=['lbv'], writes=['lbv'])
    P.op('dve', lambda: V.tensor_scalar(lb, lb, cst[:, 10:11], None, ALU.mult), reads=['lbv', 'cst'], writes=['lbv'])
    P.op('dve', lambda: V.tensor_scalar(oml, lb, -1.0, 1.0, ALU.mult, ALU.add), reads=['lbv'], writes=['lbv'])
    P.op('dve', lambda: V.tensor_scalar(noml, oml, -1.0, None, ALU.mult), reads=['lbv'], writes=['lbv'])
    P.op('dve', lambda: V.tensor_scalar(nsink[:], cst[:, 2:10], -1.0, None, ALU.mult), reads=['cst'], writes=['nsink'])
    P.op('dve', lambda: V.memset(krot[:, 0:128], 0.0), writes=['krot'])
    P.op('dve', lambda: V.memset(vA[:, 0, :], 0.0), writes=[('vA', 0)])
    P.op('dve', lambda: V.memset(S[:], 0.0), writes=[('S', h) for h in range(4)])
    P.op('dve', lambda: V.memset(Sb[:], 0.0), writes=[('Sb', h, p) for h in range(4) for p in range(2)])
    for i in range(2):
        P.op('pool', lambda: G.memset(ATm[i][:], 0.0), writes=[('ATm', i)])
    P.op('pool', lambda: G.memset(qhz[:], 0.0), writes=[('qhz', h) for h in range(4)])

    wcnt = [0]
    acnt = [0]
    C1 = 6.28125
    C2 = 2 * math.pi - C1

    def load_w(blk):
        i = wcnt[0] % NWB
        wcnt[0] += 1
        P.dma('pool', wb[i][:], w[blk], writes=[('wb', i)])
        return i

    def proj_fm(blk):
        wi = load_w(blk)
        r = (0, 2)[acnt[0] % 2]
        acnt[0] += 1
        ps, pk = pb[r], BK(r)
        for c in range(KC):
            P.op('pe', lambda: PE.matmul(ps[:], wb[wi][:, c, :], hs[:, c, :], start=(c == 0), stop=(c == KC - 1)),
                 reads=[('wb', wi), 'hs'], writes=[pk])
        return ps, pk

    tmcnt = [0]

    def proj_tm(wi, tb, ncols=128):
        r = (7, 6)[tmcnt[0] % 2]
        tmcnt[0] += 1
        ps, pk = pb[r][:, 0:ncols], BK(r)
        for c in range(KC):
            P.op('pe', lambda: PE.matmul(ps, hs[:, c, tb * 128:(tb + 1) * 128], wb[wi][:, c, 0:ncols], start=(c == 0), stop=(c == KC - 1)),
                 reads=[('wb', wi), 'hs'], writes=[pk])
        return ps, pk

    def sin_table(dst, dkey, shift, t_ang, t_k, kk):
        x, k = tmp[8], tmp[9]
        K = ['rot']
        dv = lambda fn, rd=(), wr=(): P.op('dve', fn, reads=K + list(rd), writes=K + list(wr))
        dv(lambda: V.tensor_scalar(x[:], t_ang[:], shift, None, ALU.add), rd=[kk])
        dv(lambda: V.tensor_scalar(k[:], x[:], 1.0 / (2 * math.pi), None, ALU.mult))
        dv(lambda: V.tensor_copy(posi[:], k[:]), wr=['posi'])
        dv(lambda: V.tensor_copy(k[:], posi[:]), rd=['posi'])
        dv(lambda: V.scalar_tensor_tensor(x[:], k[:], -C1, x[:], ALU.mult, ALU.add))
        dv(lambda: V.scalar_tensor_tensor(x[:], k[:], -C2, x[:], ALU.mult, ALU.add))
        dv(lambda: V.tensor_scalar(k[:], x[:], math.pi, None, ALU.is_gt))
        dv(lambda: V.scalar_tensor_tensor(x[:], k[:], -2 * math.pi, x[:], ALU.mult, ALU.add))
        dv(lambda: V.tensor_scalar(x[:], x[:], math.pi, -math.pi, ALU.min, ALU.max))
        P.op('act', lambda: A.activation(dst[:], x[:], AF.Sin), reads=K, writes=[dkey])

    qfc = [0]
    hb3 = lambda ap: ap.rearrange("p (c t) -> p c t", t=64)

    for ti in range(NT):
        t0 = ti * TT
        if stage < 1:
            break
        P.dma('pool', hs[:], hT_v[:, :, t0:t0 + TT], writes=['hs'])
        P.dma('sp', posi[:], pos[:, t0:t0 + TT].partition_broadcast(128), writes=['posi'])
        ang = tmp[7]
        P.op('dve', lambda: V.tensor_copy(ang[:], posi[:]), reads=['posi'], writes=['ang'])
        P.op('dve', lambda: V.tensor_scalar(ang[:], ang[:], cst[:, 0:1], None, ALU.mult), reads=['ang', 'cst'], writes=['ang'])
        sin_table(sinF, 'sinF', 0.0, ang, None, 'ang')
        P.op('dve', lambda: V.tensor_scalar(sinF[:], sinF[:], cst[:, 1:2], None, ALU.mult), reads=['sinF', 'cst'], writes=['sinF'])
        sin_table(cosF, 'cosF', math.pi / 2, ang, None, 'ang')

        if stage == 1:
            continue
        DBG = 0
        for blk in range(5):
            ps, pk = proj_fm(blk)
            r = qfc[0] % 2
            qfc[0] += 1
            q_, qk = qf[r], ('qf', r)
            P.op('act', lambda: A.copy(q_[:], ps[:]), reads=[pk], writes=[qk])
            t2 = tmp[5]
            if DBG == 1:
                P.op('dve', lambda: V.tensor_tensor(t2[:], q_[:], sinF[:], ALU.mult), reads=[qk, 'sinF'], writes=[('tmp', 5)])
            else:
                P.op('pe', lambda: PE.matmul(pb[3][:], perm[:], q_[:], start=True, stop=True), reads=['perm', qk], writes=[BK(3)])
                P.op('dve', lambda: V.tensor_tensor(t2[:], pb[3][:], sinF[:], ALU.mult), reads=[BK(3), 'sinF'], writes=[('tmp', 5)])
            if DBG != 3:
                P.op('dve', lambda: V.tensor_tensor(q_[:], q_[:], cosF[:], ALU.mult), reads=[qk, 'cosF'], writes=[qk])
            else:
                P.op('pool', lambda: G.tensor_tensor(q_[:], q_[:], cosF[:], ALU.mult), reads=[qk, 'cosF'], writes=[qk])
            if blk < 4:
                P.op('pool', lambda: G.tensor_tensor(qrot[:, blk, :], q_[:], t2[:], ALU.add), reads=[qk, ('tmp', 5)], writes=[('qrot', blk)])
            else:
                P.op('pool', lambda: G.tensor_tensor(krot[:, 128:128 + TT], q_[:], t2[:], ALU.add), reads=[qk, ('tmp', 5)], writes=['krot'])
        if stage < 3:
            continue
        for hh in range(4):
            ps, pk = proj_fm(5 + hh)
            P.op('act', lambda: A.activation(qs[:, hh, :], ps[:], AF.Silu), reads=[pk], writes=[('qs', hh)])
        for hh in range(4):
            ps, pk = proj_fm(9 + hh)
            P.op('act', lambda: A.activation(sig[:, hh, :], ps[:], AF.Sigmoid), reads=[pk], writes=[('sig', hh)])
        for hh in range(4):
            wi = load_w(13 + hh)
            for tb in range(NB):
                ps, pk = proj_tm(wi, tb)
                P.op('act', lambda: A.copy(hv[:, tb, hh * 128:(hh + 1) * 128], ps), reads=[pk], writes=[('hv', tb, hh)])
        gc = 0
        for hh in range(4):
            wi = load_w(17 + hh)
            for tb in range(NB):
                ps, pk = proj_tm(wi, tb)
                g_, gk = gs[gc % 2], ('gs', gc % 2)
                gc += 1
                P.op('act', lambda: A.activation(g_[:], ps, AF.Silu), reads=[pk], writes=[gk])
                P.op('pool', lambda: G.tensor_tensor(ggw[:, tb, hh * 128:(hh + 1) * 128], g_[:], gwb[:], ALU.mult), reads=[gk, 'gwb'],
                     writes=[('ggw', tb, hh)])
        wi = load_w(21)
        for tb in range(NB):
            ps, pk = proj_tm(wi, tb, 64)
            P.op('act', lambda: A.copy(vA[:, tb + 1, :], ps), reads=[pk], writes=[('vA', tb + 1)])

        if stage < 4:
            continue
        for hh in range(4):
            o5 = (hh % 2) * 0
            T1, T2, T3, T4, T5 = (tmp[i] for i in range(5))
            K1, K2, K3, K4, K5 = (('tmp', i) for i in range(5))
            P.op('dve', lambda: V.tensor_scalar(T1[:], sig[:, hh, :], lbv[:, hh, 1:2], lbv[:, hh, 0:1], ALU.mult, ALU.add),
                 reads=[('sig', hh), 'lbv'], writes=[K1])
            P.op('act', lambda: A.activation(T1[:], T1[:], AF.Ln), reads=[K1], writes=[K1])
            P.op('pool', lambda: G.tensor_scalar(T2[:], sig[:, hh, :], lbv[:, hh, 2:3], lbv[:, hh, 1:2], ALU.mult, ALU.add),
                 reads=[('sig', hh), 'lbv'], writes=[K2])
            P.op('dve', lambda: V.tensor_tensor_scan(T3[:], smask[:], T1[:], 0.0, ALU.mult, ALU.add), reads=['smask', K1], writes=[K3])
            b3 = hb3(T3[:])
            P.op('dve', lambda: V.tensor_tensor(hb3(T4[:]), b3, b3[:, :, 31:32].broadcast_to([128, NCH, 64]), ALU.subtract),
                 reads=[K3], writes=[K4])
            P.op('act', lambda: A.activation(T5[:], T4[:], AF.Exp), reads=[K4], writes=[K5])
            P.op('pool', lambda: G.tensor_tensor(qt[:, hh, :], qs[:, hh, :], T5[:], ALU.mult), reads=[('qs', hh), K5], writes=[('qt', hh)])
            P.op('act', lambda: A.activation(T5[:], T4[:], AF.Exp, scale=-1.0), reads=[K4, K5], writes=[K5])
            P.op('dve', lambda: V.tensor_tensor(kt[:, hh, :], T2[:], T5[:], ALU.mult), reads=[K2, K5], writes=[('kt', hh)])
            P.op('act', lambda: A.activation(T5[:], T3[:], AF.Exp), reads=[K3, K5], writes=[K5])
            q4 = qs[:, hh, :].rearrange("p (b two t) -> p b two t", two=2, t=64)
            e4 = T5[:].rearrange("p (b two t) -> p b two t", two=2, t=64)
            P.op('pool', lambda: G.tensor_tensor(qhz[:, hh, :, 0, 0:64], q4[:, :, 0, :], e4[:, :, 0, :], ALU.mult), reads=[('qs', hh), K5],
                 writes=[('qhz', hh)])
            P.op('pool', lambda: G.tensor_tensor(qhz[:, hh, :, 1, 64:128], q4[:, :, 1, :], e4[:, :, 1, :], ALU.mult), reads=[('qs', hh), K5],
                 writes=[('qhz', hh)])
            P.op('dve', lambda: V.tensor_copy(dec[:, hh, :], hb3(T5[:])[:, :, 63]), reads=[K5], writes=[('dec', hh)])
            P.op('dve', lambda: V.tensor_tensor(hb3(T4[:]), b3, b3[:, :, 63:64].broadcast_to([128, NCH, 64]), ALU.subtract),
                 reads=[K3, K4], writes=[K4])
            P.op('act', lambda: A.activation(T5[:], T4[:], AF.Exp, scale=-1.0), reads=[K4, K5], writes=[K5])
            P.op('dve', lambda: V.tensor_tensor(kh[:, hh, :], T2[:], T5[:], ALU.mult), reads=[K2, K5], writes=[('kh', hh)])

        if stage < 5:
            continue
        for tb in range(NB):
            gblk = ti * NB + tb
            o_, okey = ob[gblk % 2], ('ob', gblk % 2)
            mi = 1 if gblk == 0 else 0
            for h in range(8):
                ch, base = h // 2, (h % 2) * 64
                r = h % 2
                sp_, sk = pb[(0, 2)[r]][:, 0:256], BK((0, 2)[r])
                P.op('pe', lambda: PE.matmul(sp_, qrot[base:base + 64, ch, tb * 128:(tb + 1) * 128],
                                             krot[base:base + 64, tb * 128:tb * 128 + 256], start=True, stop=False),
                     reads=[('qrot', ch), 'krot'], writes=[sk])
                P.op('pe', lambda: PE.matmul(sp_, identb[:], maskb[:, mi, :], start=False, stop=True), reads=['identb', 'maskb'], writes=[sk])
                s_ = st[r]
                stk = ('st', r)
                mx, negm, rsum, es, rden = (s_[:, i:i + 1] for i in range(5))
                P.op('dve', lambda: V.reduce_max(mx, sp_, AX.X), reads=[sk], writes=[stk])
                P.op('dve', lambda: V.tensor_scalar(negm, mx, -0.125, nsink[:, h:h + 1], ALU.mult, ALU.min), reads=[stk, 'nsink'], writes=[stk])
                p_, pkey = psb_[r], ('p', r)
                P.op('act', lambda: A.activation(p_[:], sp_, AF.Exp, bias=negm, scale=0.125, accum_out=rsum), reads=[sk, stk],
                     writes=[pkey, stk])
                P.op('act', lambda: A.activation(es, cst[:, 2 + h:3 + h], AF.Exp, bias=negm), reads=['cst', stk], writes=[stk])
                P.op('dve', lambda: V.tensor_tensor(rden, rsum, es, ALU.add), reads=[stk], writes=[stk])
                P.op('dve', lambda: V.reciprocal(rden, rden), reads=[stk], writes=[stk])
                tp, tk = pb[(1, 4)[r]][:, 0:256], BK((1, 4)[r])
                for half in range(2):
                    P.op('pe', lambda: PE.transpose(tp[:, half * 128:(half + 1) * 128], p_[:, half * 128:(half + 1) * 128], identb[:]),
                         reads=[pkey, 'identb'], writes=[tk])
                pt_, ptk = pTs[r], ('pT', r)
                P.op('act', lambda: A.copy(pt_[:].rearrange("p a b -> p (a b)"), tp), reads=[tk], writes=[ptk])
                op_, opk = pb[3][:, r * 64:(r + 1) * 64], BK(3)
                P.op('pe', lambda: PE.matmul(op_, pt_[:, 0, :], vA[:, tb, :], start=True, stop=False), reads=[ptk, ('vA', tb)], writes=[opk])
                P.op('pe', lambda: PE.matmul(op_, pt_[:, 1, :], vA[:, tb + 1, :], start=False, stop=True), reads=[ptk, ('vA', tb + 1)],
                     writes=[opk])
                P.op('dve', lambda: V.tensor_scalar(o_[:, h * 64:(h + 1) * 64], op_, rden, None, ALU.mult), reads=[opk, stk], writes=[okey])
            for hh in range(4 if stage >= 6 else 0):
                r = hh % 2
                j0, j1 = 2 * tb, 2 * tb + 1
                at, atk = pb[5][:, r * 128:(r + 1) * 128], BK(5)
                ktb = kt[:, hh, tb * 128:(tb + 1) * 128]
                P.op('pe', lambda: PE.matmul(at[:, 0:64], ktb, qt[:, hh, j0 * 64:(j0 + 1) * 64], start=True, stop=True),
                     reads=[('kt', hh), ('qt', hh)], writes=[atk])
                P.op('pe', lambda: PE.matmul(at[:, 64:128], ktb, qt[:, hh, j1 * 64:(j1 + 1) * 64], start=True, stop=True),
                     reads=[('kt', hh), ('qt', hh)], writes=[atk])
                am, amk = ATm[r], ('ATm', r)
                P.op('dve', lambda: V.tensor_tensor(am[0:64, 0:64], at[0:64, 0:64], tri[0:64, :], ALU.mult), reads=[atk, 'tri'], writes=[amk])
                P.op('dve', lambda: V.tensor_tensor(am[64:128, 64:128], at[64:128, 64:128], tri[64:128, :], ALU.mult), reads=[atk, 'tri'],
                     writes=[amk])
                ktp, ktk = pb[(1, 4)[r]][:, 256:384], BK((1, 4)[r])
                P.op('pe', lambda: PE.transpose(ktp, kh[:, hh, tb * 128:(tb + 1) * 128], identb[:]), reads=[('kh', hh), 'identb'], writes=[ktk])
                kT_, kTk = khT[r], ('khT', r)
                P.op('act', lambda: A.copy(kT_[:], ktp), reads=[ktk], writes=[kTk])
                ho, hok = pb[(6, 7)[r]][:, 0:128], BK((6, 7)[r])
                vblk = hv[:, tb, hh * 128:(hh + 1) * 128]
                P.op('pe', lambda: PE.matmul(ho, am[:], vblk, start=True, stop=False), reads=[amk, ('hv', tb, hh)], writes=[hok])
                par = 0
                P.op('pe', lambda: PE.matmul(ho, qhz[:, hh, tb, 0, :], Sb[:, hh, 0, :], start=False, stop=False),
                     reads=[('qhz', hh), ('Sb', hh, 0)], writes=[hok])
                for half, j in ((0, j0), (1, j1)):
                    kv, kvk = pb[5][:, 256 + half * 128:384 + half * 128], BK(5)
                    rows = slice(half * 64, half * 64 + 64)
                    P.op('pe', lambda: PE.matmul(kv, kT_[rows, :], hv[rows, tb, hh * 128:(hh + 1) * 128], start=True, stop=True),
                         reads=[kTk, ('hv', tb, hh)], writes=[kvk])
                    P.op('dve', lambda: V.scalar_tensor_tensor(S[:, hh, :], S[:, hh, :], dec[:, hh, j:j + 1], kv, ALU.mult, ALU.add),
                         reads=[('S', hh), ('dec', hh), kvk], writes=[('S', hh)])
                    sbp = 1 - half
                    P.op('act', lambda: A.copy(Sb[:, hh, sbp, :], S[:, hh, :]), reads=[('S', hh)], writes=[('Sb', hh, sbp)])
                    if half == 0:
                        P.op('pe', lambda: PE.matmul(ho, qhz[:, hh, tb, 1, :], Sb[:, hh, 1, :], start=False, stop=True),
                             reads=[('qhz', hh), ('Sb', hh, 1)], writes=[hok])
                s_ = st[r]
                stk = ('st', r)
                ss, rs = s_[:, 5:6], s_[:, 6:7]
                P.op('act', lambda: A.activation(junk[:], ho, AF.Square, accum_out=ss), reads=[hok], writes=['junk', stk])
                P.op('dve', lambda: V.tensor_scalar(rs, ss, 1.0 / 128, RMS_EPS, ALU.mult, ALU.add), reads=[stk], writes=[stk])
                P.op('act', lambda: A.activation(rs, rs, AF.Sqrt), reads=[stk], writes=[stk])
                P.op('dve', lambda: V.reciprocal(rs, rs), reads=[stk], writes=[stk])
                P.op('dve', lambda: V.scalar_tensor_tensor(o_[:, 512 + hh * 128:640 + hh * 128], ho, rs, ggw[:, tb, hh * 128:(hh + 1) * 128],
                                                           ALU.mult, ALU.mult), reads=[hok, stk, ('ggw', tb, hh)], writes=[okey])
            P.dma('sp', mo[t0 + tb * 128:t0 + (tb + 1) * 128, :], o_[:], reads=[okey], writes=[('mo', gblk)])
        P.op('act', lambda: A.copy(krot[:, 0:128], krot[:, TT:TT + 128]), reads=['krot'], writes=['krot'])
        P.op('act', lambda: A.copy(vA[:, 0, :], vA[:, NB, :]), reads=[('vA', NB)], writes=[('vA', 0)])
    P.finish([('mo', i) for i in range(NT * NB)])
    print("even: ins", P.n_ins, "waits", P.n_wait, "sems", P.nsem)
    ctx.close()
    return nc


def even_consts(TT=512):
    p = np.arange(128)
    pm = p % 64
    inv_freq = (ROPE_THETA ** (-np.arange(0, 16, 2, dtype=np.float32) / np.float32(16))).astype(np.float32)
    invf = np.where(pm < 16, inv_freq[pm % 8], 0.0).astype(np.float32)
    sgn = np.where(pm < 8, -1.0, np.where(pm < 16, 1.0, 0.0)).astype(np.float32)
    perm = np.zeros((128, 128), np.float32)
    for m in range(128):
        mm = m % 64
        if mm < 8:
            perm[m + 8, m] = 1.0
        elif mm < 16:
            perm[m - 8, m] = 1.0
    ident = np.eye(128, dtype=np.float32)
    q = np.arange(128)[:, None]
    k = np.arange(256)[None, :]
    allowed = (k > q) & (k <= q + 128)
    m0 = np.where(allowed, 0.0, -1e9).astype(np.float32)
    m1 = np.where(allowed & (k >= 128), 0.0, -1e9).astype(np.float32)
    masks = np.ascontiguousarray(np.stack([m0, m1], axis=1))
    s = np.arange(64)[:, None]
    t = np.arange(64)[None, :]
    tri64 = (s <= t).astype(np.float32)
    tri = np.concatenate([tri64, tri64], axis=0)
    smask = np.ones((128, TT), np.float32)
    smask[:, ::64] = 0.0
    return dict(invf=invf, sgn=sgn, perm=perm, identb=ident, masks=masks, tri=tri, smask=smask)


def even_inputs(h_b, pos_b, g, j, w_in, sinks, lb_logits, gnorm_w, consts, AW=2048, KV=256, HW=2048):
    D = h_b.shape[1]
    KC = D // 128
    cq = np.arange(512 * g, 512 * g + 512)
    ck = AW + 64 * g + np.arange(64)
    cv = AW + KV + 64 * g + np.arange(64)
    base = AW + 2 * KV
    chq = base + 512 * g + np.arange(512)
    chf = base + HW + 512 * g + np.arange(512)
    chi = base + 2 * HW + 512 * g + np.arange(512)
    chg = base + 3 * HW + 512 * g + np.arange(512)
    cols = np.concatenate([cq, ck, ck, chq, chf, chi, chg, cv, cv])
    wsl = w_in[:, cols]
    wl = np.ascontiguousarray(wsl.reshape(KC, 128, NBLK, 128).transpose(2, 1, 0, 3))
    cst = np.zeros((128, 16), np.float32)
    cst[:, 0] = consts["invf"]
    cst[:, 1] = consts["sgn"]
    cst[:, 2:10] = sinks[8 * g:8 * g + 8][None, :]
    cst[:, 10] = 1.0 if j == 1 else 0.0
    hsl = slice(512 * g, 512 * g + 512)
    l0 = lb_logits[0, hsl].reshape(4, 128).T
    l1 = lb_logits[1, hsl].reshape(4, 128).T
    lbl = np.ascontiguousarray(np.stack([l0, l1], axis=-1))
    gw = np.ascontiguousarray(np.broadcast_to(gnorm_w[None, :], (128, 128)))
    return {"hT": np.ascontiguousarray(h_b.T), "w": wl, "pos": np.ascontiguousarray(pos_b[None, :]).astype(np.int32), "cst": cst, "lbl": lbl,
            "gw": gw, "perm": consts["perm"], "identb": consts["identb"], "masks": consts["masks"], "tri": consts["tri"],
            "smask": consts["smask"]}

from concourse.bass_utils import run_bass_kernel_spmd

_PROGS = {}


def _prog(name, fn):
    if name not in _PROGS:
        _PROGS[name] = fn()
    return _PROGS[name]


def kernel(x, positions, even_w_in, even_w_out, attn_sinks, hgrn_lb_logits, hgrn_gnorm_w,
           rec_w_in, rec_conv_w, rec_conv_b, rec_gate_a_w, rec_gate_a_b, rec_gate_x_w, rec_gate_x_b,
           rec_lambda, rec_w_out, ln_mix_g, ln_mix_b, ln_ffn_g, ln_ffn_b,
           router_group_w, router_group_b, router_expert_w, router_expert_b,
           moe_w_gate, moe_w_up, moe_w_down):
    import ml_dtypes
    A_ = lambda a: np.asarray(a)
    x = A_(x).astype(np.float32)
    positions = A_(positions).astype(np.int32)
    B_, S_, D_ = x.shape
    DEPTH = A_(ln_mix_g).shape[0]
    NSL = 8 // B_
    TOK = S_ // NSL
    alpha = float((2.0 * DEPTH) ** 0.25)
    consts = even_consts()
    cores = list(range(8))
    h = x
    for layer in range(DEPTH):
        j = layer // 2
        mixed = np.empty((B_, S_, D_), dtype=ml_dtypes.bfloat16)
        if layer % 2 == 0:
            nc = _prog("even", lambda: build_even(D_, S_))
            w_in = A_(even_w_in[j])
            in_maps = [even_inputs(h[c // NSL], positions[c // NSL], c % NSL, j, w_in, A_(attn_sinks[j]), A_(hgrn_lb_logits),
                                   A_(hgrn_gnorm_w[j]), consts) for c in cores]
            res = run_bass_kernel_spmd(nc, in_maps, core_ids=cores)
            for c in cores:
                b, g = c // NSL, c % NSL
                mo = np.asarray(res.results[c]["mo"])
                mixed[b][:, 512 * g:512 * g + 512] = mo[:, 0:512]
                mixed[b][:, 2048 + 512 * g:2048 + 512 * g + 512] = mo[:, 512:1024]
            w_out = A_(even_w_out[j])
        else:
            nc = _prog("odd", lambda: build_odd(D_, S_, 4))
            in_maps = [odd_inputs(h[c // NSL], c % NSL, 4, A_(rec_w_in[j]), A_(rec_conv_w[j]), A_(rec_conv_b[j]), A_(rec_gate_a_w[j]),
                                  A_(rec_gate_a_b[j]), A_(rec_gate_x_w[j]), A_(rec_gate_x_b[j]), A_(rec_lambda[j])) for c in cores]
            res = run_bass_kernel_spmd(nc, in_maps, core_ids=cores)
            for c in cores:
                b, g = c // NSL, c % NSL
                mixed[b][:, 1024 * g:1024 * (g + 1)] = np.asarray(res.results[c]["oT"]).T
            w_out = A_(rec_w_out[j])
        del in_maps, res
        nc = _prog("b", lambda: build_b(D_, TOK, 4, 8, 384, alpha))
        W = b_weights(w_out, A_(ln_mix_g[layer]), A_(ln_mix_b[layer]), A_(ln_ffn_g[layer]), A_(ln_ffn_b[layer]),
                      A_(router_group_w[layer]), A_(router_group_b[layer]), A_(router_expert_w[layer]), A_(router_expert_b[layer]),
                      A_(moe_w_gate[layer]), A_(moe_w_up[layer]), A_(moe_w_down[layer]))
        in_maps = []
        for c in cores:
            b, s = c // NSL, c % NSL
            im = dict(W)
            im["mT"] = np.ascontiguousarray(mixed[b][s * TOK:(s + 1) * TOK].T)
            im["hT"] = np.ascontiguousarray(h[b][s * TOK:(s + 1) * TOK].T)
            in_maps.append(im)
        res = run_bass_kernel_spmd(nc, in_maps, core_ids=cores)
        hn = np.empty((B_, S_, D_), dtype=np.float32)
        for c in cores:
            b, s = c // NSL, c % NSL
            hn[b][s * TOK:(s + 1) * TOK] = np.asarray(res.results[c]["oT"]).T
        h = hn
        del in_maps, res, W
    return h
```
